# Optimizing a Trainium2 kernel written in Bass

```python
import math
import jax, jax.numpy as jnp
from jax import lax
import numpy as np

D_MODEL = 1024
BATCH = 16
SEQ = 2048
DEPTH = 1

CHUNK = 64
Q_BLOCK = 128
PLE_DIM = 256
D_FF = 4 * D_MODEL
EPS = 1e-6

GLA_HEADS = 4
GLA_DK = D_MODEL // 16
GLA_DV = D_MODEL // 8
GLA_GATE_RANK = 16
GLA_TAU = 16.0
GLA_WIDTH = GLA_HEADS * GLA_DV

MLA_HEADS = 8
MLA_NOPE = 64
MLA_ROPE = 32
MLA_V = 64
MLA_QK = MLA_NOPE + MLA_ROPE
MLA_Q_RANK = 256
MLA_KV_RANK = 128
MLA_WIDTH = MLA_HEADS * MLA_V
ROPE_THETA = 10000.0

D_MIX = GLA_WIDTH + MLA_WIDTH
IN_SPLITS = (GLA_HEADS * GLA_DK, GLA_HEADS * GLA_DK, GLA_WIDTH, GLA_GATE_RANK,
             GLA_WIDTH, MLA_Q_RANK, MLA_KV_RANK, MLA_ROPE)
D_IN = (GLA_HEADS * GLA_DK * 2 + GLA_WIDTH * 2 + GLA_GATE_RANK
        + MLA_Q_RANK + MLA_KV_RANK + MLA_ROPE)

kernel_name = "hymba_gla_mla_hybrid_block"


def rms_norm(x, g):
    xf = x.astype(jnp.float32)
    y = xf * lax.rsqrt(jnp.mean(xf * xf, axis=-1, keepdims=True) + EPS)
    return (y * g.astype(jnp.float32)).astype(x.dtype)


def split_cols(t, sizes):
    out, start = [], 0
    for s in sizes:
        out.append(t[..., start:start + s])
        start += s
    return out


def rope_cos_sin(positions):
    inv_freq = ROPE_THETA ** (-jnp.arange(0, MLA_ROPE, 2, dtype=jnp.float32) / MLA_ROPE)
    ang = positions.astype(jnp.float32)[..., None] * inv_freq
    return jnp.cos(ang)[:, :, None, :], jnp.sin(ang)[:, :, None, :]


def apply_rope(x, cos, sin):
    half = x.shape[-1] // 2
    x1, x2 = x[..., :half], x[..., half:]
    c, s = cos.astype(x.dtype), sin.astype(x.dtype)
    return jnp.concatenate([x1 * c - x2 * s, x2 * c + x1 * s], axis=-1)


def gla_chunked(q, k, v, log_a):
    out_dtype = v.dtype
    B, S, H, DK = q.shape
    DV = v.shape[-1]
    NC = S // CHUNK

    def to_chunks(t):
        return t.astype(jnp.float32).reshape(B, NC, CHUNK, H, -1).transpose(1, 0, 3, 2, 4)

    qc, kc, vc, gc = to_chunks(q), to_chunks(k), to_chunks(v), to_chunks(log_a)
    causal = jnp.tril(jnp.ones((CHUNK, CHUNK), dtype=bool))[:, :, None]

    def step(state, inp):
        qi, ki, vi, gi = inp
        b = jnp.cumsum(gi, axis=2)
        b_last = b[:, :, -1:, :]
        o_inter = jnp.einsum('bhtk,bhkv->bhtv', qi * jnp.exp(b), state)
        diff = b[:, :, :, None, :] - b[:, :, None, :, :]
        decay = jnp.exp(jnp.where(causal, diff, -jnp.inf))
        scores = jnp.einsum('bhtk,bhsk,bhtsk->bhts', qi, ki, decay)
        o_intra = jnp.einsum('bhts,bhsv->bhtv', scores, vi)
        k_dec = ki * jnp.exp(b_last - b)
        state = state * jnp.exp(b_last[:, :, 0, :, None]) + jnp.einsum('bhsk,bhsv->bhkv', k_dec, vi)
        return state, o_inter + o_intra

    state0 = jnp.zeros((B, H, DK, DV), dtype=jnp.float32)
    _, oc = lax.scan(step, state0, (qc, kc, vc, gc))
    return oc.transpose(1, 0, 3, 2, 4).reshape(B, S, H, DV).astype(out_dtype)


def mla_attention(q, k, v):
    S = q.shape[1]
    scale = 1.0 / math.sqrt(MLA_QK)
    outs = []
    for blk in range(S // Q_BLOCK):
        qs, qe = blk * Q_BLOCK, (blk + 1) * Q_BLOCK
        qb, kb, vb = q[:, qs:qe], k[:, :qe], v[:, :qe]
        logits = jnp.einsum('bqhd,bkhd->bhqk', qb, kb).astype(jnp.float32) * scale
        q_chunk = (jnp.arange(qs, qe) // CHUNK)[:, None]
        k_chunk = (jnp.arange(qe) // CHUNK)[None, :]
        logits = jnp.where(k_chunk <= q_chunk, logits, -jnp.inf)
        probs = jax.nn.softmax(logits, axis=-1).astype(vb.dtype)
        outs.append(jnp.einsum('bhqk,bkhd->bqhd', probs, vb))
    return jnp.concatenate(outs, axis=1)


def setup_inputs(seed: int = 0) -> dict:
    key = jax.random.key(seed)
    ks = jax.random.split(key, 24)
    f32 = jnp.float32

    def w(k, shape, fan_in):
        return jax.random.normal(k, shape, f32) * (fan_in ** -0.5)

    def gain(k, shape):
        return 1.0 + 0.1 * jax.random.normal(k, shape, f32)

    x = jax.random.normal(ks[0], (BATCH, SEQ, D_MODEL), f32)
    p = jax.random.normal(ks[1], (DEPTH, BATCH, SEQ, PLE_DIM), f32)
    offset = jax.random.randint(ks[2], (BATCH,), 0, 64, dtype=jnp.int32) * CHUNK
    positions = (offset[:, None] + jnp.arange(SEQ, dtype=jnp.int32)[None, :]).astype(jnp.int32)
    return {
        "x": x,
        "p": p,
        "positions": positions,
        "attn_norm": gain(ks[3], (DEPTH, D_MODEL)),
        "w_in": w(ks[4], (DEPTH, D_MODEL, D_IN), D_MODEL),
        "gla_gate_w2": w(ks[5], (DEPTH, GLA_GATE_RANK, GLA_HEADS * GLA_DK), GLA_GATE_RANK),
        "gla_gate_b": 0.1 * jax.random.normal(ks[6], (DEPTH, GLA_HEADS * GLA_DK), f32),
        "gla_out_norm": gain(ks[7], (DEPTH, GLA_DV)),
        "mla_q_norm": gain(ks[8], (DEPTH, MLA_Q_RANK)),
        "mla_w_uq": w(ks[9], (DEPTH, MLA_Q_RANK, MLA_HEADS * MLA_QK), MLA_Q_RANK),
        "mla_kv_norm": gain(ks[10], (DEPTH, MLA_KV_RANK)),
        "mla_w_ukv": w(ks[11], (DEPTH, MLA_KV_RANK, MLA_HEADS * (MLA_NOPE + MLA_V)), MLA_KV_RANK),
        "qk_norm_q": gain(ks[12], (DEPTH, MLA_QK)),
        "qk_norm_k": gain(ks[13], (DEPTH, MLA_QK)),
        "w_out": w(ks[14], (DEPTH, D_MIX, D_MODEL), D_MIX),
        "mlp_norm": gain(ks[15], (DEPTH, D_MODEL)),
        "w_mlp_up": w(ks[16], (DEPTH, D_MODEL, D_FF), D_MODEL),
        "w_mlp_down": w(ks[17], (DEPTH, D_FF, D_MODEL), D_FF),
        "ple_norm": gain(ks[18], (DEPTH, D_MODEL)),
        "w_ple_gate": w(ks[19], (DEPTH, D_MODEL, D_MODEL), D_MODEL),
        "b_ple_gate": 0.1 * jax.random.normal(ks[20], (DEPTH, D_MODEL), f32),
        "w_ple_proj": w(ks[21], (DEPTH, PLE_DIM, D_MODEL), PLE_DIM),
    }


def reference(x, p, positions, attn_norm, w_in, gla_gate_w2, gla_gate_b, gla_out_norm,
              mla_q_norm, mla_w_uq, mla_kv_norm, mla_w_ukv, qk_norm_q, qk_norm_k,
              w_out, mlp_norm, w_mlp_up, w_mlp_down, ple_norm, w_ple_gate, b_ple_gate,
              w_ple_proj):
    B, S, _ = x.shape
    cos, sin = rope_cos_sin(positions)
    h = x
    for i in range(DEPTH):
        n = rms_norm(h, attn_norm[i])
        z = n @ w_in[i]
        gq, gk, gv, g_low, g_r, c_q, c_kv, k_r = split_cols(z, IN_SPLITS)

        q_g = gq.reshape(B, S, GLA_HEADS, GLA_DK) * (GLA_DK ** -0.5)
        k_g = gk.reshape(B, S, GLA_HEADS, GLA_DK)
        v_g = gv.reshape(B, S, GLA_HEADS, GLA_DV)
        log_a = jax.nn.log_sigmoid((g_low @ gla_gate_w2[i] + gla_gate_b[i]).astype(jnp.float32)) / GLA_TAU
        log_a = log_a.reshape(B, S, GLA_HEADS, GLA_DK)
        o_g = gla_chunked(q_g, k_g, v_g, log_a)
        o_g = rms_norm(o_g, gla_out_norm[i]).reshape(B, S, GLA_WIDTH) * jax.nn.silu(g_r)

        q_m = (rms_norm(c_q, mla_q_norm[i]) @ mla_w_uq[i]).reshape(B, S, MLA_HEADS, MLA_QK)
        kv = (rms_norm(c_kv, mla_kv_norm[i]) @ mla_w_ukv[i]).reshape(B, S, MLA_HEADS, MLA_NOPE + MLA_V)
        k_nope, v_m = kv[..., :MLA_NOPE], kv[..., MLA_NOPE:]
        k_rope = jnp.broadcast_to(k_r[:, :, None, :], (B, S, MLA_HEADS, MLA_ROPE))
        k_m = jnp.concatenate([k_nope, k_rope], axis=-1)
        q_m = rms_norm(q_m, qk_norm_q[i])
        k_m = rms_norm(k_m, qk_norm_k[i])
        q_m = jnp.concatenate([q_m[..., :MLA_NOPE], apply_rope(q_m[..., MLA_NOPE:], cos, sin)], axis=-1)
        k_m = jnp.concatenate([k_m[..., :MLA_NOPE], apply_rope(k_m[..., MLA_NOPE:], cos, sin)], axis=-1)
        o_m = mla_attention(q_m, k_m, v_m).reshape(B, S, MLA_WIDTH)

        h = h + jnp.concatenate([o_g, o_m], axis=-1) @ w_out[i]

        m = rms_norm(h, mlp_norm[i])
        h = h + jnp.square(jax.nn.relu(m @ w_mlp_up[i])) @ w_mlp_down[i]

        gate = jax.nn.sigmoid(rms_norm(h, ple_norm[i]) @ w_ple_gate[i] + b_ple_gate[i])
        h = h + (p[i] @ w_ple_proj[i]) * gate
    return h
```

```python
import math
import numpy as np
from contextlib import ExitStack
import concourse.bass as bass
import concourse.mybir as mybir
from concourse.bass_utils import run_bass_kernel_spmd

F32 = mybir.dt.float32
BF16 = mybir.dt.bfloat16
I32 = mybir.dt.int32
AF = mybir.ActivationFunctionType
ALU = mybir.AluOpType
AX = mybir.AxisListType

NCORES = 8
SEQ = 2048
DM = 1024
NT = SEQ // 128
DIN = 1968
EPS = 1e-6
DEBUG = False
import os as _os
STAGE = int(_os.environ.get("KSTAGE", "0"))
MERGE = int(_os.environ.get("KMERGE", "1"))
CHUNK = int(_os.environ.get("KCHUNK", "1"))
MSENG = _os.environ.get("KMSENG", "pool")
STOPK = int(_os.environ.get("KSTOPK", "-1"))
PFX = int(_os.environ.get("KPFX", "-1"))
AFX = int(_os.environ.get("KAFX", "-1"))


class _Stop(Exception):
    pass


def _cp(k):
    if STAGE == k:
        raise _Stop()


class Res:
    __slots__ = ("name", "lw", "rd")

    def __init__(self, name="r"):
        self.name = name
        self.lw = None
        self.rd = {}


class Chan:
    __slots__ = ("name", "count", "last")

    def __init__(self, name):
        self.name = name
        self.count = 0
        self.last = None


class Op:
    __slots__ = ("eng", "fn", "reads", "writes", "chan", "deps", "signal",
                 "sigval", "semkey", "waits", "idx", "barrier")

    def __init__(self, eng, fn, reads, writes, chan):
        self.eng = eng
        self.fn = fn
        self.reads = reads
        self.writes = writes
        self.chan = chan
        self.deps = []
        self.signal = chan is not None
        self.sigval = 0
        self.semkey = None
        self.waits = []
        self.barrier = False


ENGS = ("pe", "act", "dve", "pool", "sp")


class Sched:
    def __init__(self):
        self.ops = []
        self.cur = self.ops
        self.chans = []
        self.nid = 0

    def capture(self, fn):
        saved = self.cur
        self.cur = []
        fn()
        out = self.cur
        self.cur = saved
        return out

    @staticmethod
    def merge(a, b, chunk=None):
        chunk = chunk or CHUNK
        out = []
        i = j = 0
        while i < len(a) or j < len(b):
            if j >= len(b) or (i < len(a) and i * len(b) <= j * len(a)):
                out.extend(a[i:i + chunk])
                i += chunk
            else:
                out.extend(b[j:j + chunk])
                j += chunk
        return out

    def chan(self, name):
        c = Chan(name)
        self.chans.append(c)
        return c

    def add(self, eng, fn, reads=(), writes=(), chan=None):
        op = Op(eng, fn, tuple(reads), tuple(writes), chan)
        op.idx = self.nid
        self.nid += 1
        self.cur.append(op)
        return op

    def barrier(self):
        op = Op("sp", None, (), (), None)
        op.barrier = True
        op.idx = self.nid
        self.nid += 1
        self.cur.append(op)

    def finalize(self):
        last_of = {}
        pend = []
        need = set()
        for op in self.ops:
            if op.barrier:
                pend = list(last_of.values()) + [c.last for c in self.chans if c.last is not None]
                need = set(ENGS)
                continue
            deps = {}
            raw = set()
            for r in op.reads:
                if r.lw is not None:
                    deps[id(r.lw)] = r.lw
                    raw.add(id(r.lw))
            for w in op.writes:
                if w.lw is not None:
                    deps[id(w.lw)] = w.lw
                    raw.add(id(w.lw))
                for d in w.rd.values():
                    deps[id(d)] = d
            if op.chan is not None and op.chan.last is not None:
                d = op.chan.last
                deps[id(d)] = d
                raw.add(id(d))
            if op.eng in need:
                need.discard(op.eng)
                for d in pend:
                    if d.chan is None and d.eng == op.eng and op.chan is None:
                        continue
                    deps[id(d)] = d
                    raw.add(id(d))
            for k, d in deps.items():
                if d is op:
                    continue
                if d.chan is None and op.chan is None and d.eng == op.eng:
                    if op.eng == "pe":
                        continue
                    if k not in raw:
                        continue
                op.deps.append(d)
                d.signal = True
            for r in op.reads:
                key = op.eng if op.chan is None else ("dma", op.idx)
                r.rd[key] = op
            for w in op.writes:
                w.lw = op
                w.rd = {}
            if op.chan is not None:
                op.chan.last = op
            else:
                last_of[op.eng] = op
        self.ops = [o for o in self.ops if not o.barrier]
        cnt = {e: 0 for e in ENGS}
        for op in self.ops:
            if op.chan is not None:
                op.chan.count += 1
                op.sigval = 16 * op.chan.count
                op.semkey = op.chan
            elif op.signal:
                cnt[op.eng] += 1
                op.sigval = cnt[op.eng]
                op.semkey = op.eng
        seen = {e: {} for e in ENGS}
        for op in self.ops:
            needw = {}
            for d in op.deps:
                k = d.semkey
                if d.sigval > needw.get(k, 0):
                    needw[k] = d.sigval
            s = seen[op.eng]
            for k, v in needw.items():
                if v > s.get(k, 0):
                    s[k] = v
                    op.waits.append((k, v))
        self.counts = cnt

    def emit(self, nc, sems):
        per = {e: [op for op in self.ops if op.eng == e] for e in ENGS}

        def run(engname, engobj):
            for op in per[engname]:
                for k, v in op.waits:
                    engobj.wait_ge(sems[k], v)
                ins = op.fn(engobj)
                if op.signal:
                    ins.then_inc(sems[op.semkey], 16 if op.chan is not None else 1)
            if engname == "sp":
                for c in self.chans:
                    if c.count:
                        engobj.wait_ge(sems[c], 16 * c.count)

        with nc.Block() as block:
            @block.tensor
            def _(e):
                run("pe", e)

            @block.scalar
            def _(e):
                run("act", e)

            @block.vector
            def _(e):
                run("dve", e)

            @block.gpsimd
            def _(e):
                run("pool", e)

            @block.sync
            def _(e):
                run("sp", e)


class Buf:
    __slots__ = ("ap", "r")

    def __init__(self, ap, name="b"):
        self.ap = ap
        self.r = Res(name)


class Ring:
    def __init__(self, bufs):
        self.b = bufs
        self.i = 0

    def nxt(self):
        b = self.b[self.i % len(self.b)]
        self.i += 1
        return b


C_AN, C_MN, C_PN, C_QN, C_KVN, C_INVN = 0, 8, 16, 24, 26, 28
C_GQ, C_GK, C_GON, C_INVF = 32, 128, 224, 352
C_ID, C_TRIU, C_LST, C_W2 = 368, 496, 624, 752
CP = 1008


def build_nc():
    nc = bass.Bass("TRN2", target_bir_lowering=False)

    def din(name, shape, dt=F32):
        return nc.dram_tensor(name, shape, dt, kind="ExternalInput").ap()

    x = din("x", [2, SEQ, DM])
    p_in = din("p", [2, SEQ, 256])
    posl = din("posl", [128, 2, 16], I32)
    cpk = din("cpack", [128, CP])
    bple_in = din("bple", [1, 1024])
    w_in = din("w_in", [DM, DIN])
    w_uq = din("w_uq", [256, 768])
    w_ukv = din("w_ukv", [128, 1024])
    w_out = din("w_out", [DM, DM])
    w_up = din("w_up", [DM, 4096])
    w_dn = din("w_dn", [4096, DM])
    w_pg = din("w_pg", [DM, DM])
    w_pp = din("w_pp", [256, DM])
    y = nc.dram_tensor("y", [2, SEQ, DM], F32, kind="ExternalOutput").ap()
    mixd = nc.dram_tensor("mixd", [2, SEQ, DM], BF16,
                          kind="ExternalOutput" if DEBUG else "Internal").ap()

    S = Sched()
    es = ExitStack()
    with es:
        def sbt(name, shape, dt):
            return es.enter_context(nc.sbuf_tensor(name, shape, dt))

        cp = sbt("cp", [128, CP], F32)
        identb = sbt("identb", [128, 128], BF16)
        onesb = sbt("onesb", [128, 128], BF16)
        bplb = sbt("bplb", [1, 1024], BF16)
        posi = sbt("posi", [128, 2, 16], I32)
        epsb = sbt("epsb", [128, 1], F32)
        small = sbt("small", [128, 512], F32)
        ARN = 48640
        arena = sbt("arena", [128, ARN], F32)
        banks = [Buf(es.enter_context(nc.psum_tensor(f"bank{i}", [128, 512], F32))[:], f"bank{i}")
                 for i in range(8)]

        r_cp = Res("cp")
        r_id = Res("identb")
        r_ones = Res("onesb")
        r_bpl = Res("bplb")
        r_pos = Res("posi")

        def MM(out, lhsT, rhs, start, stop, rd, wr, skip=False):
            if skip:
                S.add("pe", lambda e: e.matmul(out, lhsT=lhsT, rhs=rhs, start=start, stop=stop,
                                               skip_group_check=True), rd, wr)
            else:
                S.add("pe", lambda e: e.matmul(out, lhsT=lhsT, rhs=rhs, start=start, stop=stop), rd, wr)

        def TR(out, in_, rd, wr):
            S.add("pe", lambda e: e.transpose(out=out, in_=in_, identity=identb[:]), list(rd) + [r_id], wr)

        def ACT(out, in_, func, rd, wr, **kw):
            S.add("act", lambda e: e.activation(out=out, in_=in_, func=func, **kw), rd, wr)

        def TT(eng, out, in0, in1, op, rd, wr):
            S.add(eng, lambda e: e.tensor_tensor(out=out, in0=in0, in1=in1, op=op), rd, wr)

        def TS(eng, out, in0, s1, s2, op0, op1, rd, wr):
            if op1 is None:
                S.add(eng, lambda e: e.tensor_scalar(out=out, in0=in0, scalar1=s1, scalar2=None, op0=op0), rd, wr)
            else:
                S.add(eng, lambda e: e.tensor_scalar(out=out, in0=in0, scalar1=s1, scalar2=s2, op0=op0, op1=op1), rd, wr)

        def STT(eng, out, in0, scalar, in1, op0, op1, rd, wr):
            S.add(eng, lambda e: e.scalar_tensor_tensor(out=out, in0=in0, scalar=scalar, in1=in1, op0=op0, op1=op1), rd, wr)

        def CPY(eng, out, in_, rd, wr):
            S.add(eng, lambda e: e.tensor_copy(out=out, in_=in_), rd, wr)

        def RED(out, in_, rd, wr):
            S.add("dve", lambda e: e.tensor_reduce(out=out, in_=in_, axis=AX.X, op=ALU.add), rd, wr)

        def RCP(out, in_, rd, wr):
            S.add("dve", lambda e: e.reciprocal(out=out, in_=in_), rd, wr)

        def MS(eng, ap, val, wr):
            S.add(eng, lambda e: e.memset(ap, val), (), wr)

        dma_id = [0]

        def DMA(q, out, in_, rd, wr, chan=None):
            if chan is None:
                chan = S.chan(f"d{dma_id[0]}")
                dma_id[0] += 1
            elif isinstance(chan, Ring):
                chan = chan.nxt()
            S.add(q, lambda e: e.dma_start(out=out, in_=in_), rd, wr, chan=chan)

        def chring(n, name):
            return Ring([S.chan(f"{name}{i}") for i in range(n)])

        ch_x = chring(2, "chx")
        ch_mixst = chring(4, "chms")
        ch_h = chring(4, "chh")
        ch_mixl = chring(3, "chml")
        ch_wout = chring(8, "chwo")
        ch_wup = chring(2, "chwu")
        ch_wdn = chring(2, "chwd")
        ch_p = chring(3, "chp")
        ch_y = chring(4, "chy")

        def rstd_inplace(ap, r, scale):
            ACT(ap, ap, AF.Ln, [r, r_eps], [r], scale=scale, bias=epsb[:, 0:1])
            ACT(ap, ap, AF.Exp, [r], [r], scale=-0.5)

        sm_i = [0]

        def smalloc(n):
            n8 = n
            if sm_i[0] + n8 > 512:
                sm_i[0] = 0
            a = small[:, sm_i[0]:sm_i[0] + n]
            sm_i[0] += n8
            return Buf(a, "sm")

        class Arena:
            def __init__(self):
                self.off = 0

            def take(self, shape, dt, name):
                n = int(np.prod(shape))
                nf = (n + 1) // 2 if dt == BF16 else n
                nf = (nf + 7) // 8 * 8
                assert self.off + nf <= ARN, (name, self.off, nf)
                v = arena[:, self.off:self.off + nf]
                self.off += nf
                if dt == BF16:
                    v = v.bitcast(BF16)
                v = v[:, 0:n]
                if len(shape) == 2:
                    v = v.rearrange("p (a b) -> p a b", a=shape[0])
                elif len(shape) == 3:
                    v = v.rearrange("p (a b c) -> p a b c", a=shape[0], b=shape[1])
                return Buf(v, name)

            def raw(self, nf):
                assert self.off + nf <= ARN
                v = arena[:, self.off:self.off + nf]
                self.off += nf
                return v

            def ring(self, k, shape, dt, name):
                return Ring([self.take(shape, dt, f"{name}{i}") for i in range(k)])

        RINGS = {"TPB": Ring(banks[0:1]), "RB": Ring(banks[1:4]), "SB": Ring(banks[4:6]), "ACB": Ring(banks[6:8])}

        class _RP:
            def __init__(self, k):
                self.k = k

            def nxt(self):
                return RINGS[self.k].nxt()

        TPB, RB, SB, ACB = _RP("TPB"), _RP("RB"), _RP("SB"), _RP("ACB")

        def bf8(bank):
            return bank.ap.bitcast(BF16).rearrange("p (a b) -> p a b", a=8)

        DMA("sp", cp[:], cpk[:, :], [], [r_cp])
        DMA("sp", posi[:], posl[:, :, :], [], [r_pos])
        CPY("dve", identb[:], cp[:, C_ID:C_ID + 128], [r_cp], [r_id])
        MS("pool", onesb[:], 1.0, [r_ones])
        r_eps = Res("eps")
        MS("dve", epsb[:], EPS, [r_eps])
        DMA("pool", bplb[0:1, :], bple_in[:, :], [], [r_bpl])

        g_an = cp[:, C_AN:C_AN + 8]
        g_mn = cp[:, C_MN:C_MN + 8]
        g_pn = cp[:, C_PN:C_PN + 8]
        triu = cp[:, C_TRIU:C_TRIU + 128]
        lstrict = cp[:, C_LST:C_LST + 128]
        w2ext = cp[0:32, C_W2:C_W2 + 256]
        gq_b = cp[:, C_GQ:C_GQ + 96]
        gk_b = cp[:, C_GK:C_GK + 96]
        gon_b = cp[:, C_GON:C_GON + 128]
        invf = cp[:, C_INVF:C_INVF + 16]

        A = Arena()
        win = A.take([8, DIN], BF16, "win")
        wuq = A.take([2, 768], BF16, "wuq")
        wukv = A.take([1024], BF16, "wukv")
        kTc = A.take([8, SEQ], BF16, "kT")
        Vc = A.take([NT, 8, 65], BF16, "Vc")
        nT_raw = A.raw(2048)
        nT = Buf(nT_raw.bitcast(BF16).rearrange("p (a b) -> p a b", a=8), "nT")
        angb = Buf(nT_raw[:, 0:1024].rearrange("p (a b c) -> p a b c", a=4, b=NT), "ang")
        angb.r = nT.r
        angi = Buf(nT_raw[:, 1024:1280].rearrange("p (a b) -> p a b", a=NT), "angi")
        angi.r = nT.r
        qTg_r = A.ring(2, [8, 512], BF16, "qT")
        cqT = A.take([2, 512], BF16, "cqT")
        ckvT = A.take([512], BF16, "ckvT")
        xn_r = A.ring(2, [1024], BF16, "xn")
        vtm_r = A.ring(2, [512], BF16, "vtm")
        qeT_r = A.ring(2, [2, 128], BF16, "qeT")
        kdT_r = A.ring(2, [2, 2, 128], BF16, "kdT")
        kdec_r = A.ring(2, [256], BF16, "kdec")
        ATm_r = A.ring(2, [4, 128], BF16, "ATm")
        Sbf_r = A.ring(2, [2, 2, 128], BF16, "Sbf")
        qn_b = A.take([8, 96], BF16, "qn")
        kn_b = A.take([8, 96], BF16, "kn")
        pb_r = A.ring(3, [512], BF16, "pb")
        mixTM_r = A.ring(2, [4, 1024], BF16, "mixTM")
        sqj = A.take([1024], BF16, "sqj")
        xt_r = A.ring(2, [1024], F32, "xt")
        gqT = A.take([2, 512], BF16, "gqT")
        gkT = A.take([2, 512], BF16, "gkT")
        glx = A.take([512], F32, "glx")
        sp_r = A.ring(2, [256], F32, "sp")
        eb_b = A.take([2, 128], F32, "eb")
        einv_b = A.take([2, 128], F32, "einv")
        erb_b = A.take([256], F32, "erb")
        gktm_r = A.ring(2, [256], F32, "gktm")
        sge_r = A.ring(2, [512], F32, "sge")
        sq32 = A.take([768], F32, "sq32")
        og32 = A.take([4, 128], F32, "og32")
        qtmp = A.take([8, 96], F32, "qtmp")
        ktmp = A.take([8, 64], F32, "ktmp")
        rtmp = A.take([4, 8, 16], F32, "rtmp")
        krt = A.take([6, 32], F32, "krt")
        Sst = A.take([2, 128], F32, "S")
        cosT = A.take([NT, 16], F32, "cos")
        sinT = A.take([NT, 16], F32, "sin")
        kT_res = [Res(f"kT{i}") for i in range(NT)]
        Vc_res = [Res(f"Vc{i}") for i in range(NT)]
        print("phase A arena use (fp32 cols):", A.off, "of", ARN)

        w_in_v = w_in.rearrange("(kc p) n -> p kc n", p=128)
        for kc in range(8):
            DMA("pool", win.ap[:, kc, :], w_in_v[:, kc, :], [], [win.r])
        DMA("pool", wuq.ap, w_uq.rearrange("(j p) n -> p j n", p=128), [], [wuq.r])
        DMA("pool", wukv.ap, w_ukv[:, :], [], [wukv.r])
        MS("pool", Vc.ap, 1.0, Vc_res)
        MS("pool", glx.ap, 1.0, [glx.r])
        for b_ in kdT_r.b + Sbf_r.b:
            MS("pool", b_.ap, 0.0, [b_.r])

        angi_i = angi.ap.bitcast(I32)

        def trig(dst, src_ap, src_r):
            TS("dve", angi_i, src_ap, float(1.0 / (2 * math.pi)), None, ALU.mult, None, [src_r], [angi.r])
            kf = angb.ap[:, 2, :, :]
            CPY("dve", kf, angi_i, [angi.r], [angb.r])
            rr = angb.ap[:, 3, :, :]
            STT("dve", rr, kf, float(-2 * math.pi), src_ap, ALU.mult, ALU.add, [angb.r, src_r], [angb.r])
            TS("dve", kf, rr, float(math.pi), None, ALU.is_gt, None, [angb.r], [angb.r])
            STT("dve", rr, kf, float(-2 * math.pi), rr, ALU.mult, ALU.add, [angb.r], [angb.r])
            TS("dve", rr, rr, 3.14159, -3.14159, ALU.min, ALU.max, [angb.r], [angb.r])
            ACT(dst.ap, rr, AF.Sin, [angb.r], [dst.r])

        seqst = {}
        gctx = {}

        def seq_setup(s):
            posf = smalloc(16)
            CPY("dve", posf.ap, posi[:, s, :], [r_pos], [posf.r])
            a0 = angb.ap[:, 0, :, :]
            a1 = angb.ap[:, 1, :, :]
            TT("dve", a0, posf.ap[:, :, None].to_broadcast([128, NT, 16]),
               invf[:, None, :].to_broadcast([128, NT, 16]), ALU.mult, [posf.r, r_cp], [angb.r])
            TS("dve", a1, a0, float(math.pi / 2), None, ALU.add, None, [angb.r], [angb.r])
            trig(sinT, a0, angb.r)
            trig(cosT, a1, angb.r)
            MS("dve", Sst.ap, 0.0, [Sst.r])
            sbf0 = Sbf_r.nxt()
            MS("pool", sbf0.ap, 0.0, [sbf0.r])
            seqst[s] = {"Sbf": [sbf0], "xt_q": []}
            load_x(s, 0)

        def load_x(s, i):
            b = xt_r.nxt()
            DMA("sp", b.ap, x[s, i * 128:(i + 1) * 128, :], [], [b.r], ch_x)
            seqst[s]["xt_q"].append(b)

        def prep_a(s, g, merged):
            if merged:
                RINGS["TPB"] = Ring(banks[0:1])
                RINGS["RB"] = Ring(banks[1:3])
            else:
                RINGS["TPB"] = Ring(banks[0:2])
                RINGS["RB"] = Ring(banks[2:8])
            xt_q = seqst[s]["xt_q"]
            if True:
                for t in range(4):
                    i = 4 * g + t
                    if i + 1 < NT:
                        load_x(s, i + 1)
                    xb = xt_q.pop(0)
                    ss = smalloc(1)
                    ACT(sqj.ap, xb.ap, AF.Square, [xb.r], [sqj.r, ss.r], accum_out=ss.ap)
                    rstd_inplace(ss.ap, ss.r, 1.0 / DM)
                    xn = xn_r.nxt()
                    TS("dve", xn.ap, xb.ap, ss.ap, None, ALU.mult, None, [xb.r, ss.r], [xn.r])
                    tp = TPB.nxt()
                    tpv = bf8(tp)
                    for kc in range(8):
                        TR(tpv[:, kc, :], xn.ap[:, kc * 128:(kc + 1) * 128], [xn.r], [tp.r])
                    TT("dve", nT.ap[:, :, t * 128:(t + 1) * 128], tpv,
                       g_an[:, :, None].to_broadcast([128, 8, 128]), ALU.mult, [tp.r, r_cp], [nT.r])
                fm = [("gq", 0, 0, 128), ("gq", 1, 128, 128), ("gk", 0, 256, 128), ("gk", 1, 384, 128),
                      ("gl", 0, 1024, 16), ("cq", 0, 1552, 128), ("cq", 1, 1680, 128), ("ckv", 0, 1808, 128)]
                for (nm, j, c0, m) in fm:
                    bk = RB.nxt()
                    for kc in range(8):
                        MM(bk.ap[0:m, :], win.ap[:, kc, c0:c0 + m], nT.ap[:, kc, :], kc == 0, kc == 7,
                           [win.r, nT.r], [bk.r])
                    if nm == "gq":
                        ACT(gqT.ap[:, j, :], bk.ap, AF.Copy, [bk.r], [gqT.r])
                    elif nm == "gk":
                        ACT(gkT.ap[:, j, :], bk.ap, AF.Copy, [bk.r], [gkT.r])
                    elif nm == "gl":
                        CPY("dve", glx.ap[0:16, :], bk.ap[0:16, :], [bk.r], [glx.r])
                    elif nm == "cq":
                        TS("dve", cqT.ap[:, j, :], bk.ap, cp[:, C_QN + j:C_QN + j + 1], None, ALU.mult, None,
                           [bk.r, r_cp], [cqT.r])
                    else:
                        TS("dve", ckvT.ap, bk.ap, cp[:, C_KVN:C_KVN + 1], None, ALU.mult, None,
                           [bk.r, r_cp], [ckvT.r])
        def prep_b(s, g):
            RINGS["TPB"] = Ring(banks[0:2])
            RINGS["RB"] = Ring(banks[2:8])
            Sbf_cur = seqst[s]["Sbf"]
            qTg = qTg_r.nxt()
            mixTM = mixTM_r.nxt()
            gctx[(s, g)] = (qTg, mixTM)
            if True:
                for t in range(4):
                    i = 4 * g + t
                    c0 = t * 128
                    c1 = c0 + 128

                    def tmproj(n0, n):
                        bk = RB.nxt()
                        for kc in range(8):
                            MM(bk.ap[:, 0:n], nT.ap[:, kc, c0:c1], win.ap[:, kc, n0:n0 + n], kc == 0, kc == 7,
                               [win.r, nT.r], [bk.r])
                        return bk

                    b_gk = tmproj(256, 256)
                    gktm = gktm_r.nxt()
                    ACT(gktm.ap, b_gk.ap[:, 0:256], AF.Copy, [b_gk.r], [gktm.r])
                    b_gv = tmproj(512, 512)
                    vtm = vtm_r.nxt()
                    ACT(vtm.ap, b_gv.ap, AF.Copy, [b_gv.r], [vtm.r])
                    b_gr = tmproj(1040, 512)
                    sge = sge_r.nxt()
                    ACT(sge.ap, b_gr.ap, AF.Exp, [b_gr.r], [sge.r], scale=-1.0)
                    TS("dve", sge.ap, sge.ap, 1.0, None, ALU.add, None, [sge.r], [sge.r])
                    RCP(sge.ap, sge.ap, [sge.r], [sge.r])
                    TT("dve", sge.ap, sge.ap, b_gr.ap, ALU.mult, [sge.r, b_gr.r], [sge.r])
                    b_c = tmproj(1552, 416)
                    st = smalloc(4)
                    ACT(sq32.ap[:, 0:256], b_c.ap[:, 0:256], AF.Square, [b_c.r], [sq32.r, st.r], accum_out=st.ap[:, 0:1])
                    ACT(sq32.ap[:, 256:384], b_c.ap[:, 256:384], AF.Square, [b_c.r], [sq32.r, st.r], accum_out=st.ap[:, 1:2])
                    ACT(sq32.ap[:, 384:416], b_c.ap[:, 384:416], AF.Square, [b_c.r], [sq32.r, st.r], accum_out=st.ap[:, 2:3])
                    kr = krt.ap[:, 0, :]
                    CPY("dve", kr, b_c.ap[:, 384:416], [b_c.r], [krt.r])

                    b_la = RB.nxt()
                    MM(b_la.ap[:, 0:256], glx.ap[0:32, c0:c1], w2ext, True, True, [glx.r, r_cp], [b_la.r])
                    spb = sp_r.nxt()
                    ACT(spb.ap, b_la.ap[:, 0:256], AF.Exp, [b_la.r], [spb.r], scale=-1.0)
                    ACT(spb.ap, spb.ap, AF.Ln, [spb.r], [spb.r], bias=1.0)
                    b_c2 = RB.nxt()
                    for hp in range(2):
                        MM(b_c2.ap[:, hp * 128:(hp + 1) * 128], spb.ap[:, hp * 128:(hp + 1) * 128], triu, True, True,
                           [spb.r, r_cp], [b_c2.r])
                    MM(b_c2.ap[:, 256:512], lstrict, spb.ap, True, True, [spb.r, r_cp], [b_c2.r])
                    c2v = b_c2.ap[:, 0:256].rearrange("p (a b) -> p a b", a=2)
                    ACT(eb_b.ap, c2v, AF.Exp, [b_c2.r], [eb_b.r], scale=-1.0 / 16)
                    ACT(einv_b.ap, c2v, AF.Exp, [b_c2.r], [einv_b.r], scale=1.0 / 16)
                    ACT(erb_b.ap, b_c2.ap[:, 256:512], AF.Exp, [b_c2.r], [erb_b.r], scale=-1.0 / 16)
                    qeT = qeT_r.nxt()
                    kdT = kdT_r.nxt()
                    kdec = kdec_r.nxt()
                    STT("dve", qeT.ap, gqT.ap[:, :, c0:c1], 0.125, eb_b.ap, ALU.mult, ALU.mult,
                        [gqT.r, eb_b.r], [qeT.r])
                    for r in range(2):
                        sl = slice(r * 64, (r + 1) * 64)
                        TT("dve", kdT.ap[sl, :, r, :], gkT.ap[sl, :, c0:c1], einv_b.ap[sl, :, :], ALU.mult,
                           [gkT.r, einv_b.r], [kdT.r])
                    TT("dve", kdec.ap, gktm.ap, erb_b.ap, ALU.mult, [gktm.r, erb_b.r], [kdec.r])
                    b_at = RB.nxt()
                    atv = b_at.ap.rearrange("p (a b) -> p a b", a=4)
                    for h in range(4):
                        hp, r = h // 2, h % 2
                        MM(atv[:, h, :], kdT.ap[:, hp, r, :], qeT.ap[:, hp, :],
                           True, True, [kdT.r, qeT.r], [b_at.r])
                    ATm = ATm_r.nxt()
                    TT("dve", ATm.ap, atv, triu[:, None, :].to_broadcast([128, 4, 128]), ALU.mult,
                       [b_at.r, r_cp], [ATm.r])
                    b_o = RB.nxt()
                    ov = b_o.ap.rearrange("p (a b) -> p a b", a=4)
                    sbf = Sbf_cur[0]
                    for h in range(4):
                        hp, r = h // 2, h % 2
                        MM(ov[:, h, :], ATm.ap[:, h, :], vtm.ap[:, h * 128:(h + 1) * 128], True, False,
                           [ATm.r, vtm.r], [b_o.r])
                        MM(ov[:, h, :], qeT.ap[:, hp, :], sbf.ap[:, hp, r, :],
                           False, True, [qeT.r, sbf.r], [b_o.r])
                    b_w = RB.nxt()
                    wv = b_w.ap.rearrange("p (a b) -> p a b", a=4)
                    for h in range(4):
                        hp = h // 2
                        MM(wv[:, h, :], kdec.ap[:, hp * 128:(hp + 1) * 128], vtm.ap[:, h * 128:(h + 1) * 128],
                           True, True, [kdec.r, vtm.r], [b_w.r])
                    for h in range(4):
                        hp, r = h // 2, h % 2
                        sl = slice(r * 64, (r + 1) * 64)
                        STT("dve", Sst.ap[sl, hp, :], Sst.ap[sl, hp, :], eb_b.ap[sl, hp, 127:128], wv[sl, h, :],
                            ALU.mult, ALU.add, [Sst.r, eb_b.r, b_w.r], [Sst.r])
                    sbn = Sbf_r.nxt()
                    for r in range(2):
                        sl = slice(r * 64, (r + 1) * 64)
                        CPY("pool", sbn.ap[sl, :, r, :], Sst.ap[sl, :, :], [Sst.r], [sbn.r])
                    Sbf_cur[0] = sbn
                    ssg = smalloc(4)
                    ACT(sq32.ap[:, 0:512], b_o.ap, AF.Square, [b_o.r], [sq32.r])
                    RED(ssg.ap, sq32.ap[:, 0:512].rearrange("p (a b) -> p a b", a=4), [sq32.r], [ssg.r])
                    rstd_inplace(ssg.ap, ssg.r, 1.0 / 128)
                    TT("dve", og32.ap, ov, ssg.ap[:, :, None].to_broadcast([128, 4, 128]), ALU.mult,
                       [b_o.r, ssg.r], [og32.r])
                    TT("pool", og32.ap, og32.ap, gon_b[:, None, :].to_broadcast([128, 4, 128]), ALU.mult,
                       [og32.r, r_cp], [og32.r])
                    TT("pool", mixTM.ap[:, t, 0:512], og32.ap.rearrange("p a b -> p (a b)"), sge.ap, ALU.mult,
                       [og32.r, sge.r], [mixTM.r])

                    TT("dve", st.ap[:, 0:2], st.ap[:, 0:2], cp[:, C_INVN:C_INVN + 2], ALU.mult, [st.r, r_cp], [st.r])
                    ACT(st.ap[:, 0:2], st.ap[:, 0:2], AF.Ln, [st.r, r_eps], [st.r], bias=epsb[:, 0:1])
                    ACT(st.ap[:, 0:2], st.ap[:, 0:2], AF.Exp, [st.r], [st.r], scale=-0.5)
                    rsq = st.ap[:, 0:1]
                    rskv = st.ap[:, 1:2]
                    b_q0 = RB.nxt()
                    b_q1 = RB.nxt()
                    for j in range(2):
                        MM(b_q0.ap[:, 0:480], cqT.ap[:, j, c0:c1], wuq.ap[:, j, 0:480], j == 0, j == 1,
                           [cqT.r, wuq.r], [b_q0.r])
                    for j in range(2):
                        MM(b_q1.ap[:, 0:288], cqT.ap[:, j, c0:c1], wuq.ap[:, j, 480:768], j == 0, j == 1,
                           [cqT.r, wuq.r], [b_q1.r])
                    q0v = b_q0.ap[:, 0:480].rearrange("p (a b) -> p a b", a=5)
                    q1v = b_q1.ap[:, 0:288].rearrange("p (a b) -> p a b", a=3)
                    ACT(sq32.ap[:, 0:480], b_q0.ap[:, 0:480], AF.Square, [b_q0.r], [sq32.r])
                    ACT(sq32.ap[:, 480:768], b_q1.ap[:, 0:288], AF.Square, [b_q1.r], [sq32.r])
                    sq8 = smalloc(8)
                    RED(sq8.ap, sq32.ap.rearrange("p (a b) -> p a b", a=8), [sq32.r], [sq8.r])
                    c1s = smalloc(1)
                    TS("dve", c1s.ap, rsq, rsq, 1.0 / 96, ALU.mult, ALU.mult, [st.r], [c1s.r])
                    ACT(sq8.ap, sq8.ap, AF.Ln, [sq8.r, c1s.r, r_eps], [sq8.r], scale=c1s.ap, bias=epsb[:, 0:1])
                    ACT(sq8.ap, sq8.ap, AF.Exp, [sq8.r], [sq8.r], scale=-0.5)
                    TS("dve", sq8.ap, sq8.ap, rsq, float(96 ** -0.5), ALU.mult, ALU.mult, [sq8.r, st.r], [sq8.r])
                    TT("dve", qtmp.ap[:, 0:5, :], q0v, gq_b[:, None, :].to_broadcast([128, 5, 96]), ALU.mult,
                       [b_q0.r, r_cp], [qtmp.r])
                    TT("dve", qtmp.ap[:, 5:8, :], q1v, gq_b[:, None, :].to_broadcast([128, 3, 96]), ALU.mult,
                       [b_q1.r, r_cp], [qtmp.r])
                    TT("pool", qtmp.ap, qtmp.ap, sq8.ap[:, :, None].to_broadcast([128, 8, 96]), ALU.mult,
                       [qtmp.r, sq8.r], [qtmp.r])
                    CPY("pool", qn_b.ap[:, :, 0:64], qtmp.ap[:, :, 0:64], [qtmp.r], [qn_b.r])
                    cb = cosT.ap[:, i, :][:, None, :].to_broadcast([128, 8, 16])
                    sb_ = sinT.ap[:, i, :][:, None, :].to_broadcast([128, 8, 16])
                    x1 = qtmp.ap[:, :, 64:80]
                    x2 = qtmp.ap[:, :, 80:96]
                    r0, r1, r2, r3 = (rtmp.ap[:, k, :, :] for k in range(4))
                    TT("pool", r0, x1, cb, ALU.mult, [qtmp.r, cosT.r], [rtmp.r])
                    TT("pool", r1, x2, sb_, ALU.mult, [qtmp.r, sinT.r], [rtmp.r])
                    TT("pool", r2, x2, cb, ALU.mult, [qtmp.r, cosT.r], [rtmp.r])
                    TT("pool", r3, x1, sb_, ALU.mult, [qtmp.r, sinT.r], [rtmp.r])
                    TT("pool", qn_b.ap[:, :, 64:80], r0, r1, ALU.subtract, [rtmp.r], [qn_b.r])
                    TT("pool", qn_b.ap[:, :, 80:96], r2, r3, ALU.add, [rtmp.r], [qn_b.r])
                    b_k0 = RB.nxt()
                    b_k1 = RB.nxt()
                    MM(b_k0.ap, ckvT.ap[:, c0:c1], wukv.ap[:, 0:512], True, True, [ckvT.r, wukv.r], [b_k0.r])
                    MM(b_k1.ap, ckvT.ap[:, c0:c1], wukv.ap[:, 512:1024], True, True, [ckvT.r, wukv.r], [b_k1.r])
                    k0v = b_k0.ap.rearrange("p (a b) -> p a b", a=4)
                    k1v = b_k1.ap.rearrange("p (a b) -> p a b", a=4)
                    sqk = sq32.ap[:, 0:512].rearrange("p (a b) -> p a b", a=8)
                    ACT(sqk[:, 0:4, :], k0v[:, :, 0:64], AF.Square, [b_k0.r], [sq32.r])
                    ACT(sqk[:, 4:8, :], k1v[:, :, 0:64], AF.Square, [b_k1.r], [sq32.r])
                    sk8 = smalloc(8)
                    RED(sk8.ap, sqk, [sq32.r], [sk8.r])
                    c2s = smalloc(1)
                    TS("dve", c2s.ap, rskv, rskv, None, ALU.mult, None, [st.r], [c2s.r])
                    TS("dve", sk8.ap, sk8.ap, c2s.ap, st.ap[:, 2:3], ALU.mult, ALU.add, [sk8.r, c2s.r, st.r], [sk8.r])
                    ACT(sk8.ap, sk8.ap, AF.Ln, [sk8.r, r_eps], [sk8.r], scale=1.0 / 96, bias=epsb[:, 0:1])
                    ACT(sk8.ap, sk8.ap, AF.Exp, [sk8.r], [sk8.r], scale=-0.5)
                    fkn = smalloc(8)
                    TS("dve", fkn.ap, sk8.ap, rskv, None, ALU.mult, None, [sk8.r, st.r], [fkn.r])
                    TT("dve", ktmp.ap[:, 0:4, :], k0v[:, :, 0:64], gk_b[:, None, 0:64].to_broadcast([128, 4, 64]),
                       ALU.mult, [b_k0.r, r_cp], [ktmp.r])
                    TT("dve", ktmp.ap[:, 4:8, :], k1v[:, :, 0:64], gk_b[:, None, 0:64].to_broadcast([128, 4, 64]),
                       ALU.mult, [b_k1.r, r_cp], [ktmp.r])
                    TT("pool", kn_b.ap[:, :, 0:64], ktmp.ap, fkn.ap[:, :, None].to_broadcast([128, 8, 64]), ALU.mult,
                       [ktmp.r, fkn.r], [kn_b.r])
                    krg = krt.ap[:, 1, :]
                    TT("pool", krg, kr, gk_b[:, 64:96], ALU.mult, [krt.r, r_cp], [krt.r])
                    c16 = cosT.ap[:, i, :]
                    s16 = sinT.ap[:, i, :]
                    t0, t1, t2, t3 = (krt.ap[:, 2 + k // 2, (k % 2) * 16:(k % 2) * 16 + 16] for k in range(4))
                    krr = krt.ap[:, 4, :]
                    TT("pool", t0, krg[:, 0:16], c16, ALU.mult, [krt.r, cosT.r], [krt.r])
                    TT("pool", t1, krg[:, 16:32], s16, ALU.mult, [krt.r, sinT.r], [krt.r])
                    TT("pool", t2, krg[:, 16:32], c16, ALU.mult, [krt.r, cosT.r], [krt.r])
                    TT("pool", t3, krg[:, 0:16], s16, ALU.mult, [krt.r, sinT.r], [krt.r])
                    TT("pool", krr[:, 0:16], t0, t1, ALU.subtract, [krt.r], [krt.r])
                    TT("pool", krr[:, 16:32], t2, t3, ALU.add, [krt.r], [krt.r])
                    TT("pool", kn_b.ap[:, :, 64:96], krr[:, None, :].to_broadcast([128, 8, 32]),
                       sk8.ap[:, :, None].to_broadcast([128, 8, 32]), ALU.mult, [krt.r, sk8.r], [kn_b.r])
                    ACT(Vc.ap[:, i, 0:4, 0:64], k0v[:, :, 64:128], AF.Copy, [b_k0.r, st.r], [Vc_res[i]], scale=rskv)
                    ACT(Vc.ap[:, i, 4:8, 0:64], k1v[:, :, 64:128], AF.Copy, [b_k1.r, st.r], [Vc_res[i]], scale=rskv)
                    tq = TPB.nxt()
                    tqv = bf8(tq)
                    for h in range(8):
                        TR(tqv[0:96, h, :], qn_b.ap[:, h, :], [qn_b.r], [tq.r])
                    CPY("dve", qTg.ap[0:96, :, c0:c1], tqv[0:96, :, :], [tq.r], [qTg.r])
                    tk = TPB.nxt()
                    tkv = bf8(tk)
                    for h in range(8):
                        TR(tkv[0:96, h, :], kn_b.ap[:, h, :], [kn_b.r], [tk.r])
                    ACT(kTc.ap[0:96, :, i * 128:(i + 1) * 128], tkv[0:96, :, :], AF.Copy, [tk.r], [kT_res[i]])

        def attention(s, g, merged=False):
            if merged:
                RINGS["SB"] = Ring(banks[3:6])
                RINGS["ACB"] = Ring(banks[6:8])
            else:
                RINGS["SB"] = Ring(banks[0:4])
                RINGS["ACB"] = Ring(banks[4:6])
            qTg, mixTM = gctx[(s, g)]
            if True:
                nk = 4 * g + 4
                items = [(h, kt) for h in range(8) for kt in range(nk)]
                accs = {}

                def qk(h, kt):
                    lo = max(0, kt - 4 * g)
                    bs = SB.nxt()
                    MM(bs.ap[:, lo * 128:512], kTc.ap[0:96, h, kt * 128:(kt + 1) * 128],
                       qTg.ap[0:96, h, lo * 128:512], True, True, [kT_res[kt], qTg.r], [bs.r])
                    pbf = pb_r.nxt()
                    ACT(pbf.ap[:, lo * 128:512], bs.ap[:, lo * 128:512], AF.Exp, [bs.r], [pbf.r])
                    if kt >= 4 * g:
                        MS(MSENG, pbf.ap[64:128, lo * 128:lo * 128 + 64], 0.0, [pbf.r])
                    return pbf

                def pv(h, kt, pbf):
                    lo = max(0, kt - 4 * g)
                    if kt == 0:
                        accs[h] = ACB.nxt()
                    acc = accs[h]
                    accv = acc.ap[:, 0:260].rearrange("p (a b) -> p a b", a=4)
                    for qt in range(lo, 4):
                        MM(accv[:, qt, :], pbf.ap[:, qt * 128:(qt + 1) * 128], Vc.ap[:, kt, h, :],
                           kt == 0 and qt == 0, kt == 4 * g + qt, [pbf.r, Vc_res[kt]], [acc.r], skip=True)
                    if kt == nk - 1:
                        rec = smalloc(4)
                        RCP(rec.ap, accv[:, :, 64], [acc.r], [rec.r])
                        TT("dve", mixTM.ap[:, :, 512 + h * 64:512 + (h + 1) * 64], accv[:, :, 0:64],
                           rec.ap[:, :, None].to_broadcast([128, 4, 64]), ALU.mult, [acc.r, rec.r], [mixTM.r])

                LA = 2
                pq = [qk(*items[m]) for m in range(min(LA, len(items)))]
                for n in range(len(items)):
                    if n + LA < len(items):
                        pq.append(qk(*items[n + LA]))
                    pv(items[n][0], items[n][1], pq.pop(0))
                for t in range(4):
                    i = 4 * g + t
                    DMA("sp", mixd[s, i * 128:(i + 1) * 128, :], mixTM.ap[:, t, :], [mixTM.r], [], ch_mixst)

        order = [(sq_, g_) for sq_ in range(2) for g_ in range(4)]

        seq_setup(0)
        prep_a(0, 0, False)
        prep_b(0, 0)
        for k in range(len(order)):
            nxt_ = order[k + 1] if k + 1 < len(order) else None
            if MERGE and nxt_ is not None and nxt_[1] != 0:
                a_ops = S.capture(lambda: attention(order[k][0], order[k][1], True))
                p_ops = S.capture(lambda: prep_a(nxt_[0], nxt_[1], True))
                S.cur.extend(S.merge(a_ops, p_ops))
                prep_b(*nxt_)
            else:
                attention(order[k][0], order[k][1], False)
                if nxt_ is not None:
                    if nxt_[1] == 0:
                        seq_setup(nxt_[0])
                    prep_a(nxt_[0], nxt_[1], False)
                    prep_b(*nxt_)

        S.barrier()

        RINGS["TPB"] = Ring(banks[0:2])
        RINGS["RB"] = Ring(banks[2:8])
        B = Arena()
        wout = B.take([8, 1024], BF16, "wout")
        wpp = B.take([2, 1024], BF16, "wpp")
        wup_r = B.ring(2, [8, 512], BF16, "wup")
        wdn_r = B.ring(2, [4, 1024], BF16, "wdn")
        mT = B.take([8, SEQ], BF16, "mT")
        hid_r = B.ring(2, [4, 512], BF16, "hid")
        xnb_r = B.ring(2, [1024], BF16, "xnb")
        mixl_r = B.ring(3, [1024], BF16, "mixl")
        mixT_r = B.ring(2, [8, 128], BF16, "mixT")
        hT_r = B.ring(3, [8, 128], BF16, "hT")
        pbf_r = B.ring(3, [256], BF16, "pbf")
        pT_r = B.ring(3, [2, 128], BF16, "pT")
        sqjb = B.take([1024], BF16, "sqjb")
        hres = B.take([NT, 1024], F32, "h")
        rl_r = B.ring(2, [512], F32, "rl")
        eg_r = B.ring(2, [512], F32, "eg")
        eg_r = Ring(eg_r.b + rl_r.b)
        print("phase B arena use (fp32 cols):", B.off, "of", ARN)
        h_r = [Res(f"h{i}") for i in range(NT)]

        w_up_v = w_up.rearrange("(kc p) f -> p kc f", p=128)
        w_dn_v = w_dn.rearrange("(fc p) n -> p fc n", p=128)
        DMA("pool", wpp.ap, w_pp.rearrange("(j p) n -> p j n", p=128), [], [wpp.r])

        def norm_stats(i):
            ss = smalloc(1)
            ACT(sqjb.ap, hres.ap[:, i, :], AF.Square, [h_r[i]], [sqjb.r, ss.r], accum_out=ss.ap)
            rstd_inplace(ss.ap, ss.r, 1.0 / DM)
            return ss

        def norm_apply(i, ss, gain, dstT, dst_r, dst_cols):
            xn = xnb_r.nxt()
            TS("dve", xn.ap, hres.ap[:, i, :], ss.ap, None, ALU.mult, None, [h_r[i], ss.r], [xn.r])
            tp = TPB.nxt()
            tpv = bf8(tp)
            for kc in range(8):
                TR(tpv[:, kc, :], xn.ap[:, kc * 128:(kc + 1) * 128], [xn.r], [tp.r])
            TT("dve", dstT[:, :, dst_cols], tpv, gain[:, :, None].to_broadcast([128, 8, 128]), ALU.mult,
               [tp.r, r_cp], [dst_r])

        def load_chunk(e8):
            wu = wup_r.nxt()
            wd = wdn_r.nxt()
            DMA("pool", wu.ap, w_up_v[:, :, e8 * 512:(e8 + 1) * 512], [], [wu.r], ch_wup)
            DMA("pool", wd.ap, w_dn_v[:, 4 * e8:4 * e8 + 4, :], [], [wd.r], ch_wdn)
            return (wu, wd)

        nxt_chunk = [None]

        def phaseB_seq(s):
            nxt_chunk[0] = load_chunk(0)
            for kc in range(8):
                DMA("pool", wout.ap[:, kc, :], w_out.rearrange("(kc p) n -> p kc n", p=128)[:, kc, :], [], [wout.r], ch_wout)
            b0 = {}

            b0l = {}

            def b0_0(i):
                DMA("sp", hres.ap[:, i, :], x[s, i * 128:(i + 1) * 128, :], [], [h_r[i]], ch_h)
                ml = mixl_r.nxt()
                DMA("sp", ml.ap, mixd[s, i * 128:(i + 1) * 128, :], [], [ml.r], ch_mixl)
                b0l[i] = ml

            def b0_1(i):
                ml = b0l[i]
                tp = TPB.nxt()
                tpv = bf8(tp)
                for kc in range(8):
                    TR(tpv[:, kc, :], ml.ap[:, kc * 128:(kc + 1) * 128], [ml.r], [tp.r])
                mxT = mixT_r.nxt()
                ACT(mxT.ap, tpv, AF.Copy, [tp.r], [mxT.r])
                b0[i] = mxT

            def b0_2(i):
                mxT = b0[i]
                for half in range(2):
                    bk = RB.nxt()
                    for kc in range(8):
                        MM(bk.ap, mxT.ap[:, kc, :], wout.ap[:, kc, half * 512:(half + 1) * 512], kc == 0, kc == 7,
                           [mxT.r, wout.r], [bk.r])
                    hs = hres.ap[:, i, half * 512:(half + 1) * 512]
                    TT("dve", hs, hs, bk.ap, ALU.add, [h_r[i], bk.r], [h_r[i]])

            def b0_3(i):
                b0[i] = norm_stats(i)

            def b0_4(i):
                norm_apply(i, b0[i], g_mn, mT.ap, mT.r, slice(i * 128, (i + 1) * 128))

            for step in range(NT + 5):
                for st_, fn_ in ((0, b0_0), (2, b0_1), (3, b0_2), (4, b0_3), (5, b0_4)):
                    i = step - st_
                    if 0 <= i < NT:
                        fn_(i)
            for e8 in range(8):
                wu, wd = nxt_chunk[0]
                if e8 + 1 < 8:
                    nxt_chunk[0] = load_chunk(e8 + 1)
                if e8 == 0:
                    for kc in range(8):
                        DMA("pool", wout.ap[:, kc, :], w_pg.rearrange("(kc p) n -> p kc n", p=128)[:, kc, :],
                            [], [wout.r], ch_wout)
                for g in range(4):
                    hb = hid_r.nxt()
                    for fc in range(4):
                        bk = RB.nxt()
                        for kc in range(8):
                            MM(bk.ap, wu.ap[:, kc, fc * 128:(fc + 1) * 128], mT.ap[:, kc, g * 512:(g + 1) * 512],
                               kc == 0, kc == 7, [wu.r, mT.r], [bk.r])
                        rl = rl_r.nxt()
                        ACT(rl.ap, bk.ap, AF.Relu, [bk.r], [rl.r])
                        TT("dve", hb.ap[:, fc, :], rl.ap, rl.ap, ALU.mult, [rl.r], [hb.r])
                    for tt in range(4):
                        i = 4 * g + tt
                        for half in range(2):
                            bk = RB.nxt()
                            for fc in range(4):
                                MM(bk.ap, hb.ap[:, fc, tt * 128:(tt + 1) * 128],
                                   wd.ap[:, fc, half * 512:(half + 1) * 512], fc == 0, fc == 3, [hb.r, wd.r], [bk.r])
                            hs = hres.ap[:, i, half * 512:(half + 1) * 512]
                            TT("dve", hs, hs, bk.ap, ALU.add, [h_r[i], bk.r], [h_r[i]])
            pl = {}

            def ple_1(i):
                pb_ = pbf_r.nxt()
                DMA("pool", pb_.ap, p_in[s, i * 128:(i + 1) * 128, :], [], [pb_.r], ch_p)
                pl[i] = [norm_stats(i), pb_]

            def ple_2(i):
                hT = hT_r.nxt()
                pb_ = pl[i][1]
                norm_apply(i, pl[i][0], g_pn, hT.ap, hT.r, slice(0, 128))
                tp = TPB.nxt()
                tpv = bf8(tp)
                for j in range(2):
                    TR(tpv[:, j, :], pb_.ap[:, j * 128:(j + 1) * 128], [pb_.r], [tp.r])
                pT = pT_r.nxt()
                ACT(pT.ap, tpv[:, 0:2, :], AF.Copy, [tp.r], [pT.r])
                pl[i] = [hT, pT]

            def ple_3(i):
                hT, pT = pl[i][0], pl[i][1]
                egs = []
                for half in range(2):
                    hsl = slice(half * 512, (half + 1) * 512)
                    bg = RB.nxt()
                    MM(bg.ap, onesb[0:1, :], bplb[0:1, hsl], True, False, [r_ones, r_bpl], [bg.r])
                    for kc in range(8):
                        MM(bg.ap, hT.ap[:, kc, :], wout.ap[:, kc, hsl], False, kc == 7, [hT.r, wout.r], [bg.r])
                    eg = eg_r.nxt()
                    ACT(eg.ap, bg.ap, AF.Exp, [bg.r], [eg.r], scale=-1.0)
                    ACT(eg.ap, eg.ap, AF.Ln, [eg.r], [eg.r], bias=1.0)
                    ACT(eg.ap, eg.ap, AF.Exp, [eg.r], [eg.r], scale=-1.0)
                    egs.append(eg)
                pl[i] = [hT, pT, egs]

            def ple_4(i):
                hT, pT, egs = pl[i]
                for half in range(2):
                    hsl = slice(half * 512, (half + 1) * 512)
                    eg = egs[half]
                    bp = RB.nxt()
                    for j in range(2):
                        MM(bp.ap, pT.ap[:, j, :], wpp.ap[:, j, hsl], j == 0, j == 1, [pT.r, wpp.r], [bp.r])
                    TT("dve", eg.ap, eg.ap, bp.ap, ALU.mult, [eg.r, bp.r], [eg.r])
                    hs = hres.ap[:, i, hsl]
                    TT("pool", hs, hs, eg.ap, ALU.add, [h_r[i], eg.r], [h_r[i]])
                DMA("sp", y[s, i * 128:(i + 1) * 128, :], hres.ap[:, i, :], [h_r[i]], [], ch_y)

            for step in range(NT + 4):
                for st_, fn_ in ((0, ple_1), (2, ple_2), (3, ple_3), (4, ple_4)):
                    i = step - st_
                    if 0 <= i < NT:
                        fn_(i)

        for s in range(2):
            if STOPK >= 0:
                break
            phaseB_seq(s)

        S.finalize()
        print("ops:", len(S.ops), "signals:", S.counts, "chans:", len(S.chans))
        sems = {}
        for k in ENGS:
            sems[k] = es.enter_context(nc.semaphore(f"s_{k}"))
        for ci, c in enumerate(S.chans):
            sems[c] = es.enter_context(nc.semaphore(f"c{ci}"))
        S.emit(nc, sems)
    return nc


def make_cpack(attn_norm, mlp_norm, ple_norm, mla_q_norm, mla_kv_norm, qk_norm_q, qk_norm_k,
               gla_out_norm, gla_gate_w2, gla_gate_b, b_ple_gate):
    c = np.zeros((128, CP), np.float32)
    c[:, C_AN:C_AN + 8] = attn_norm.reshape(8, 128).T
    c[:, C_MN:C_MN + 8] = mlp_norm.reshape(8, 128).T
    c[:, C_PN:C_PN + 8] = ple_norm.reshape(8, 128).T
    c[:, C_QN:C_QN + 2] = mla_q_norm.reshape(2, 128).T
    c[:, C_KVN] = mla_kv_norm.reshape(128)
    c[:, C_INVN] = 1.0 / 256
    c[:, C_INVN + 1] = 1.0 / 128
    c[:, C_GQ:C_GQ + 96] = qk_norm_q.reshape(1, 96)
    c[:, C_GK:C_GK + 96] = qk_norm_k.reshape(1, 96)
    c[:, C_GON:C_GON + 128] = gla_out_norm.reshape(1, 128)
    invf = (10000.0 ** (-np.arange(0, 32, 2, dtype=np.float32) / np.float32(32))).astype(np.float32)
    c[:, C_INVF:C_INVF + 16] = invf[None, :]
    c[:, C_ID:C_ID + 128] = np.eye(128, dtype=np.float32)
    sidx = np.arange(128)[:, None]
    tidx = np.arange(128)[None, :]
    c[:, C_TRIU:C_TRIU + 128] = (sidx <= tidx).astype(np.float32)
    c[:, C_LST:C_LST + 128] = (sidx > tidx).astype(np.float32)
    c[0:16, C_W2:C_W2 + 256] = gla_gate_w2.reshape(16, 256)
    c[16, C_W2:C_W2 + 256] = gla_gate_b.reshape(256)
    return c


_NC_CACHE = {}


def kernel(x, p, positions, attn_norm, w_in, gla_gate_w2, gla_gate_b, gla_out_norm,
           mla_q_norm, mla_w_uq, mla_kv_norm, mla_w_ukv, qk_norm_q, qk_norm_k,
           w_out, mlp_norm, w_mlp_up, w_mlp_down, ple_norm, w_ple_gate, b_ple_gate,
           w_ple_proj):
    f = lambda a: np.ascontiguousarray(np.asarray(a, dtype=np.float32))
    x = f(x)
    p = f(p)[0]
    positions = np.ascontiguousarray(np.asarray(positions, dtype=np.int32))
    cpack = make_cpack(f(attn_norm), f(mlp_norm), f(ple_norm), f(mla_q_norm), f(mla_kv_norm),
                       f(qk_norm_q), f(qk_norm_k), f(gla_out_norm), f(gla_gate_w2), f(gla_gate_b),
                       f(b_ple_gate))
    shared = {
        "cpack": cpack, "bple": f(b_ple_gate).reshape(1, 1024),
        "w_in": f(w_in)[0], "w_uq": f(mla_w_uq)[0], "w_ukv": f(mla_w_ukv)[0], "w_out": f(w_out)[0],
        "w_up": f(w_mlp_up)[0], "w_dn": f(w_mlp_down)[0], "w_pg": f(w_ple_gate)[0], "w_pp": f(w_ple_proj)[0],
    }
    in_maps = []
    for c in range(NCORES):
        pos2 = positions[2 * c:2 * c + 2]
        posl = np.ascontiguousarray(pos2.reshape(2, NT, 128).transpose(2, 0, 1))
        m = dict(shared)
        m["x"] = np.ascontiguousarray(x[2 * c:2 * c + 2])
        m["p"] = np.ascontiguousarray(p[2 * c:2 * c + 2])
        m["posl"] = posl
        in_maps.append(m)
    if "nc" not in _NC_CACHE:
        _NC_CACHE["nc"] = build_nc()
    nc = _NC_CACHE["nc"]
    res = run_bass_kernel_spmd(nc, in_maps, core_ids=list(range(NCORES)))
    _NC_CACHE["last"] = res
    out = np.concatenate([np.asarray(r["y"]) for r in res.results], axis=0)
    return out.astype(np.float32)
```

```python
import math
import numpy as np
from contextlib import ExitStack
import concourse.bass as bass
import concourse.mybir as mybir
from concourse.bass_utils import run_bass_kernel_spmd

F32 = mybir.dt.float32
BF16 = mybir.dt.bfloat16
I32 = mybir.dt.int32
AF = mybir.ActivationFunctionType
ALU = mybir.AluOpType
AX = mybir.AxisListType

NCORES = 8
SEQ = 2048
DM = 1024
NT = SEQ // 128
DIN = 1968
EPS = 1e-6
DEBUG = False
import os as _os
STAGE = int(_os.environ.get("KSTAGE", "0"))
MERGE = int(_os.environ.get("KMERGE", "1"))
CHUNK = int(_os.environ.get("KCHUNK", "1"))
MSENG = _os.environ.get("KMSENG", "pool")
STOPK = int(_os.environ.get("KSTOPK", "-1"))
PFX = int(_os.environ.get("KPFX", "-1"))
AFX = int(_os.environ.get("KAFX", "-1"))


class _Stop(Exception):
    pass


def _cp(k):
    if STAGE == k:
        raise _Stop()


class Res:
    __slots__ = ("name", "lw", "rd")

    def __init__(self, name="r"):
        self.name = name
        self.lw = None
        self.rd = {}


class Chan:
    __slots__ = ("name", "count", "last")

    def __init__(self, name):
        self.name = name
        self.count = 0
        self.last = None


class Op:
    __slots__ = ("eng", "fn", "reads", "writes", "chan", "deps", "signal",
                 "sigval", "semkey", "waits", "idx", "barrier")

    def __init__(self, eng, fn, reads, writes, chan):
        self.eng = eng
        self.fn = fn
        self.reads = reads
        self.writes = writes
        self.chan = chan
        self.deps = []
        self.signal = chan is not None
        self.sigval = 0
        self.semkey = None
        self.waits = []
        self.barrier = False


ENGS = ("pe", "act", "dve", "pool", "sp")


class Sched:
    def __init__(self):
        self.ops = []
        self.cur = self.ops
        self.chans = []
        self.nid = 0

    def capture(self, fn):
        saved = self.cur
        self.cur = []
        fn()
        out = self.cur
        self.cur = saved
        return out

    @staticmethod
    def merge(a, b, chunk=None):
        chunk = chunk or CHUNK
        out = []
        i = j = 0
        while i < len(a) or j < len(b):
            if j >= len(b) or (i < len(a) and i * len(b) <= j * len(a)):
                out.extend(a[i:i + chunk])
                i += chunk
            else:
                out.extend(b[j:j + chunk])
                j += chunk
        return out

    def chan(self, name):
        c = Chan(name)
        self.chans.append(c)
        return c

    def add(self, eng, fn, reads=(), writes=(), chan=None):
        op = Op(eng, fn, tuple(reads), tuple(writes), chan)
        op.idx = self.nid
        self.nid += 1
        self.cur.append(op)
        return op

    def barrier(self):
        op = Op("sp", None, (), (), None)
        op.barrier = True
        op.idx = self.nid
        self.nid += 1
        self.cur.append(op)

    def finalize(self):
        last_of = {}
        pend = []
        need = set()
        for op in self.ops:
            if op.barrier:
                pend = list(last_of.values()) + [c.last for c in self.chans if c.last is not None]
                need = set(ENGS)
                continue
            deps = {}
            raw = set()
            for r in op.reads:
                if r.lw is not None:
                    deps[id(r.lw)] = r.lw
                    raw.add(id(r.lw))
            for w in op.writes:
                if w.lw is not None:
                    deps[id(w.lw)] = w.lw
                    raw.add(id(w.lw))
                for d in w.rd.values():
                    deps[id(d)] = d
            if op.chan is not None and op.chan.last is not None:
                d = op.chan.last
                deps[id(d)] = d
                raw.add(id(d))
            if op.eng in need:
                need.discard(op.eng)
                for d in pend:
                    if d.chan is None and d.eng == op.eng and op.chan is None:
                        continue
                    deps[id(d)] = d
                    raw.add(id(d))
            for k, d in deps.items():
                if d is op:
                    continue
                if d.chan is None and op.chan is None and d.eng == op.eng:
                    if op.eng == "pe":
                        continue
                    if k not in raw:
                        continue
                op.deps.append(d)
                d.signal = True
            for r in op.reads:
                key = op.eng if op.chan is None else ("dma", op.idx)
                r.rd[key] = op
            for w in op.writes:
                w.lw = op
                w.rd = {}
            if op.chan is not None:
                op.chan.last = op
            else:
                last_of[op.eng] = op
        self.ops = [o for o in self.ops if not o.barrier]
        cnt = {e: 0 for e in ENGS}
        for op in self.ops:
            if op.chan is not None:
                op.chan.count += 1
                op.sigval = 16 * op.chan.count
                op.semkey = op.chan
            elif op.signal:
                cnt[op.eng] += 1
                op.sigval = cnt[op.eng]
                op.semkey = op.eng
        seen = {e: {} for e in ENGS}
        for op in self.ops:
            needw = {}
            for d in op.deps:
                k = d.semkey
                if d.sigval > needw.get(k, 0):
                    needw[k] = d.sigval
            s = seen[op.eng]
            for k, v in needw.items():
                if v > s.get(k, 0):
                    s[k] = v
                    op.waits.append((k, v))
        self.counts = cnt

    def emit(self, nc, sems):
        per = {e: [op for op in self.ops if op.eng == e] for e in ENGS}

        def run(engname, engobj):
            for op in per[engname]:
                for k, v in op.waits:
                    engobj.wait_ge(sems[k], v)
                ins = op.fn(engobj)
                if op.signal:
                    ins.then_inc(sems[op.semkey], 16 if op.chan is not None else 1)
            if engname == "sp":
                for c in self.chans:
                    if c.count:
                        engobj.wait_ge(sems[c], 16 * c.count)

        with nc.Block() as block:
            @block.tensor
            def _(e):
                run("pe", e)

            @block.scalar
            def _(e):
                run("act", e)

            @block.vector
            def _(e):
                run("dve", e)

            @block.gpsimd
            def _(e):
                run("pool", e)

            @block.sync
            def _(e):
                run("sp", e)


class Buf:
    __slots__ = ("ap", "r")

    def __init__(self, ap, name="b"):
        self.ap = ap
        self.r = Res(name)


class Ring:
    def __init__(self, bufs):
        self.b = bufs
        self.i = 0

    def nxt(self):
        b = self.b[self.i % len(self.b)]
        self.i += 1
        return b


C_AN, C_MN, C_PN, C_QN, C_KVN, C_INVN = 0, 8, 16, 24, 26, 28
C_GQ, C_GK, C_GON, C_INVF = 32, 128, 224, 352
C_ID, C_TRIU, C_LST, C_W2 = 368, 496, 624, 752
CP = 1008


def build_nc():
    nc = bass.Bass("TRN2", target_bir_lowering=False)

    def din(name, shape, dt=F32):
        return nc.dram_tensor(name, shape, dt, kind="ExternalInput").ap()

    x = din("x", [2, SEQ, DM])
    p_in = din("p", [2, SEQ, 256])
    posl = din("posl", [128, 2, 16], I32)
    cpk = din("cpack", [128, CP])
    bple_in = din("bple", [1, 1024])
    w_in = din("w_in", [DM, DIN])
    w_uq = din("w_uq", [256, 768])
    w_ukv = din("w_ukv", [128, 1024])
    w_out = din("w_out", [DM, DM])
    w_up = din("w_up", [DM, 4096])
    w_dn = din("w_dn", [4096, DM])
    w_pg = din("w_pg", [DM, DM])
    w_pp = din("w_pp", [256, DM])
    y = nc.dram_tensor("y", [2, SEQ, DM], F32, kind="ExternalOutput").ap()
    mixd = nc.dram_tensor("mixd", [2, SEQ, DM], BF16,
                          kind="ExternalOutput" if DEBUG else "Internal").ap()

    S = Sched()
    es = ExitStack()
    with es:
        def sbt(name, shape, dt):
            return es.enter_context(nc.sbuf_tensor(name, shape, dt))

        cp = sbt("cp", [128, CP], F32)
        identb = sbt("identb", [128, 128], BF16)
        onesb = sbt("onesb", [128, 128], BF16)
        bplb = sbt("bplb", [1, 1024], BF16)
        posi = sbt("posi", [128, 2, 16], I32)
        epsb = sbt("epsb", [128, 1], F32)
        small = sbt("small", [128, 512], F32)
        ARN = 48640
        arena = sbt("arena", [128, ARN], F32)
        banks = [Buf(es.enter_context(nc.psum_tensor(f"bank{i}", [128, 512], F32))[:], f"bank{i}")
                 for i in range(8)]

        r_cp = Res("cp")
        r_id = Res("identb")
        r_ones = Res("onesb")
        r_bpl = Res("bplb")
        r_pos = Res("posi")

        def MM(out, lhsT, rhs, start, stop, rd, wr, skip=False):
            if skip:
                S.add("pe", lambda e: e.matmul(out, lhsT=lhsT, rhs=rhs, start=start, stop=stop,
                                               skip_group_check=True), rd, wr)
            else:
                S.add("pe", lambda e: e.matmul(out, lhsT=lhsT, rhs=rhs, start=start, stop=stop), rd, wr)

        def TR(out, in_, rd, wr):
            S.add("pe", lambda e: e.transpose(out=out, in_=in_, identity=identb[:]), list(rd) + [r_id], wr)

        def ACT(out, in_, func, rd, wr, **kw):
            S.add("act", lambda e: e.activation(out=out, in_=in_, func=func, **kw), rd, wr)

        def TT(eng, out, in0, in1, op, rd, wr):
            S.add(eng, lambda e: e.tensor_tensor(out=out, in0=in0, in1=in1, op=op), rd, wr)

        def TS(eng, out, in0, s1, s2, op0, op1, rd, wr):
            if op1 is None:
                S.add(eng, lambda e: e.tensor_scalar(out=out, in0=in0, scalar1=s1, scalar2=None, op0=op0), rd, wr)
            else:
                S.add(eng, lambda e: e.tensor_scalar(out=out, in0=in0, scalar1=s1, scalar2=s2, op0=op0, op1=op1), rd, wr)

        def STT(eng, out, in0, scalar, in1, op0, op1, rd, wr):
            S.add(eng, lambda e: e.scalar_tensor_tensor(out=out, in0=in0, scalar=scalar, in1=in1, op0=op0, op1=op1), rd, wr)

        def CPY(eng, out, in_, rd, wr):
            S.add(eng, lambda e: e.tensor_copy(out=out, in_=in_), rd, wr)

        def RED(out, in_, rd, wr):
            S.add("dve", lambda e: e.tensor_reduce(out=out, in_=in_, axis=AX.X, op=ALU.add), rd, wr)

        def RCP(out, in_, rd, wr):
            S.add("dve", lambda e: e.reciprocal(out=out, in_=in_), rd, wr)

        def MS(eng, ap, val, wr):
            S.add(eng, lambda e: e.memset(ap, val), (), wr)

        dma_id = [0]

        def DMA(q, out, in_, rd, wr, chan=None):
            if chan is None:
                chan = S.chan(f"d{dma_id[0]}")
                dma_id[0] += 1
            elif isinstance(chan, Ring):
                chan = chan.nxt()
            S.add(q, lambda e: e.dma_start(out=out, in_=in_), rd, wr, chan=chan)

        def chring(n, name):
            return Ring([S.chan(f"{name}{i}") for i in range(n)])

        ch_x = chring(2, "chx")
        ch_mixst = chring(4, "chms")
        ch_h = chring(4, "chh")
        ch_mixl = chring(3, "chml")
        ch_wout = chring(8, "chwo")
        ch_wup = chring(2, "chwu")
        ch_wdn = chring(2, "chwd")
        ch_p = chring(3, "chp")
        ch_y = chring(4, "chy")

        def rstd_inplace(ap, r, scale):
            ACT(ap, ap, AF.Ln, [r, r_eps], [r], scale=scale, bias=epsb[:, 0:1])
            ACT(ap, ap, AF.Exp, [r], [r], scale=-0.5)

        sm_i = [0]

        def smalloc(n):
            n8 = n
            if sm_i[0] + n8 > 512:
                sm_i[0] = 0
            a = small[:, sm_i[0]:sm_i[0] + n]
            sm_i[0] += n8
            return Buf(a, "sm")

        class Arena:
            def __init__(self):
                self.off = 0

            def take(self, shape, dt, name):
                n = int(np.prod(shape))
                nf = (n + 1) // 2 if dt == BF16 else n
                nf = (nf + 7) // 8 * 8
                assert self.off + nf <= ARN, (name, self.off, nf)
                v = arena[:, self.off:self.off + nf]
                self.off += nf
                if dt == BF16:
                    v = v.bitcast(BF16)
                v = v[:, 0:n]
                if len(shape) == 2:
                    v = v.rearrange("p (a b) -> p a b", a=shape[0])
                elif len(shape) == 3:
                    v = v.rearrange("p (a b c) -> p a b c", a=shape[0], b=shape[1])
                return Buf(v, name)

            def raw(self, nf):
                assert self.off + nf <= ARN
                v = arena[:, self.off:self.off + nf]
                self.off += nf
                return v

            def ring(self, k, shape, dt, name):
                return Ring([self.take(shape, dt, f"{name}{i}") for i in range(k)])

        RINGS = {"TPB": Ring(banks[0:1]), "RB": Ring(banks[1:4]), "SB": Ring(banks[4:6]), "ACB": Ring(banks[6:8])}

        class _RP:
            def __init__(self, k):
                self.k = k

            def nxt(self):
                return RINGS[self.k].nxt()

        TPB, RB, SB, ACB = _RP("TPB"), _RP("RB"), _RP("SB"), _RP("ACB")

        def bf8(bank):
            return bank.ap.bitcast(BF16).rearrange("p (a b) -> p a b", a=8)

        DMA("sp", cp[:], cpk[:, :], [], [r_cp])
        DMA("sp", posi[:], posl[:, :, :], [], [r_pos])
        CPY("dve", identb[:], cp[:, C_ID:C_ID + 128], [r_cp], [r_id])
        MS("pool", onesb[:], 1.0, [r_ones])
        r_eps = Res("eps")
        MS("dve", epsb[:], EPS, [r_eps])
        DMA("pool", bplb[0:1, :], bple_in[:, :], [], [r_bpl])

        g_an = cp[:, C_AN:C_AN + 8]
        g_mn = cp[:, C_MN:C_MN + 8]
        g_pn = cp[:, C_PN:C_PN + 8]
        triu = cp[:, C_TRIU:C_TRIU + 128]
        lstrict = cp[:, C_LST:C_LST + 128]
        w2ext = cp[0:32, C_W2:C_W2 + 256]
        gq_b = cp[:, C_GQ:C_GQ + 96]
        gk_b = cp[:, C_GK:C_GK + 96]
        gon_b = cp[:, C_GON:C_GON + 128]
        invf = cp[:, C_INVF:C_INVF + 16]

        A = Arena()
        win = A.take([8, DIN], BF16, "win")
        wuq = A.take([2, 768], BF16, "wuq")
        wukv = A.take([1024], BF16, "wukv")
        kTc = A.take([8, SEQ], BF16, "kT")
        Vc = A.take([NT, 8, 65], BF16, "Vc")
        nT_raw = A.raw(2048)
        nT = Buf(nT_raw.bitcast(BF16).rearrange("p (a b) -> p a b", a=8), "nT")
        angb = Buf(nT_raw[:, 0:1024].rearrange("p (a b c) -> p a b c", a=4, b=NT), "ang")
        angb.r = nT.r
        angi = Buf(nT_raw[:, 1024:1280].rearrange("p (a b) -> p a b", a=NT), "angi")
        angi.r = nT.r
        qTg_r = A.ring(2, [8, 512], BF16, "qT")
        cqT = A.take([2, 512], BF16, "cqT")
        ckvT = A.take([512], BF16, "ckvT")
        xn_r = A.ring(2, [1024], BF16, "xn")
        vtm_r = A.ring(2, [512], BF16, "vtm")
        qeT_r = A.ring(2, [2, 128], BF16, "qeT")
        kdT_r = A.ring(2, [2, 2, 128], BF16, "kdT")
        kdec_r = A.ring(2, [256], BF16, "kdec")
        ATm_r = A.ring(2, [4, 128], BF16, "ATm")
        Sbf_r = A.ring(2, [2, 2, 128], BF16, "Sbf")
        qn_b = A.take([8, 96], BF16, "qn")
        kn_b = A.take([8, 96], BF16, "kn")
        pb_r = A.ring(3, [512], BF16, "pb")
        mixTM_r = A.ring(2, [4, 1024], BF16, "mixTM")
        sqj = A.take([1024], BF16, "sqj")
        xt_r = A.ring(2, [1024], F32, "xt")
        gqT = A.take([2, 512], BF16, "gqT")
        gkT = A.take([2, 512], BF16, "gkT")
        glx = A.take([512], F32, "glx")
        sp_r = A.ring(2, [256], F32, "sp")
        eb_b = A.take([2, 128], F32, "eb")
        einv_b = A.take([2, 128], F32, "einv")
        erb_b = A.take([256], F32, "erb")
        gktm_r = A.ring(2, [256], F32, "gktm")
        sge_r = A.ring(2, [512], F32, "sge")
        sq32 = A.take([768], F32, "sq32")
        og32 = A.take([4, 128], F32, "og32")
        qtmp = A.take([8, 96], F32, "qtmp")
        ktmp = A.take([8, 64], F32, "ktmp")
        rtmp = A.take([4, 8, 16], F32, "rtmp")
        krt = A.take([6, 32], F32, "krt")
        Sst = A.take([2, 128], F32, "S")
        cosT = A.take([NT, 16], F32, "cos")
        sinT = A.take([NT, 16], F32, "sin")
        kT_res = [Res(f"kT{i}") for i in range(NT)]
        Vc_res = [Res(f"Vc{i}") for i in range(NT)]
        print("phase A arena use (fp32 cols):", A.off, "of", ARN)

        w_in_v = w_in.rearrange("(kc p) n -> p kc n", p=128)
        for kc in range(8):
            DMA("pool", win.ap[:, kc, :], w_in_v[:, kc, :], [], [win.r])
        DMA("pool", wuq.ap, w_uq.rearrange("(j p) n -> p j n", p=128), [], [wuq.r])
        DMA("pool", wukv.ap, w_ukv[:, :], [], [wukv.r])
        MS("pool", Vc.ap, 1.0, Vc_res)
        MS("pool", glx.ap, 1.0, [glx.r])
        for b_ in kdT_r.b + Sbf_r.b:
            MS("pool", b_.ap, 0.0, [b_.r])

        angi_i = angi.ap.bitcast(I32)

        def trig(dst, src_ap, src_r):
            TS("dve", angi_i, src_ap, float(1.0 / (2 * math.pi)), None, ALU.mult, None, [src_r], [angi.r])
            kf = angb.ap[:, 2, :, :]
            CPY("dve", kf, angi_i, [angi.r], [angb.r])
            rr = angb.ap[:, 3, :, :]
            STT("dve", rr, kf, float(-2 * math.pi), src_ap, ALU.mult, ALU.add, [angb.r, src_r], [angb.r])
            TS("dve", kf, rr, float(math.pi), None, ALU.is_gt, None, [angb.r], [angb.r])
            STT("dve", rr, kf, float(-2 * math.pi), rr, ALU.mult, ALU.add, [angb.r], [angb.r])
            TS("dve", rr, rr, 3.14159, -3.14159, ALU.min, ALU.max, [angb.r], [angb.r])
            ACT(dst.ap, rr, AF.Sin, [angb.r], [dst.r])

        seqst = {}
        gctx = {}

        def seq_setup(s):
            posf = smalloc(16)
            CPY("dve", posf.ap, posi[:, s, :], [r_pos], [posf.r])
            a0 = angb.ap[:, 0, :, :]
            a1 = angb.ap[:, 1, :, :]
            TT("dve", a0, posf.ap[:, :, None].to_broadcast([128, NT, 16]),
               invf[:, None, :].to_broadcast([128, NT, 16]), ALU.mult, [posf.r, r_cp], [angb.r])
            TS("dve", a1, a0, float(math.pi / 2), None, ALU.add, None, [angb.r], [angb.r])
            trig(sinT, a0, angb.r)
            trig(cosT, a1, angb.r)
            MS("dve", Sst.ap, 0.0, [Sst.r])
            sbf0 = Sbf_r.nxt()
            MS("pool", sbf0.ap, 0.0, [sbf0.r])
            seqst[s] = {"Sbf": [sbf0], "xt_q": []}
            load_x(s, 0)

        def load_x(s, i):
            b = xt_r.nxt()
            DMA("sp", b.ap, x[s, i * 128:(i + 1) * 128, :], [], [b.r], ch_x)
            seqst[s]["xt_q"].append(b)

        def prep_a(s, g, merged):
            if merged:
                RINGS["TPB"] = Ring(banks[0:1])
                RINGS["RB"] = Ring(banks[1:3])
            else:
                RINGS["TPB"] = Ring(banks[0:2])
                RINGS["RB"] = Ring(banks[2:8])
            xt_q = seqst[s]["xt_q"]
            if True:
                for t in range(4):
                    i = 4 * g + t
                    if i + 1 < NT:
                        load_x(s, i + 1)
                    xb = xt_q.pop(0)
                    ss = smalloc(1)
                    ACT(sqj.ap, xb.ap, AF.Square, [xb.r], [sqj.r, ss.r], accum_out=ss.ap)
                    rstd_inplace(ss.ap, ss.r, 1.0 / DM)
                    xn = xn_r.nxt()
                    TS("dve", xn.ap, xb.ap, ss.ap, None, ALU.mult, None, [xb.r, ss.r], [xn.r])
                    tp = TPB.nxt()
                    tpv = bf8(tp)
                    for kc in range(8):
                        TR(tpv[:, kc, :], xn.ap[:, kc * 128:(kc + 1) * 128], [xn.r], [tp.r])
                    TT("dve", nT.ap[:, :, t * 128:(t + 1) * 128], tpv,
                       g_an[:, :, None].to_broadcast([128, 8, 128]), ALU.mult, [tp.r, r_cp], [nT.r])
                fm = [("gq", 0, 0, 128), ("gq", 1, 128, 128), ("gk", 0, 256, 128), ("gk", 1, 384, 128),
                      ("gl", 0, 1024, 16), ("cq", 0, 1552, 128), ("cq", 1, 1680, 128), ("ckv", 0, 1808, 128)]
                for (nm, j, c0, m) in fm:
                    bk = RB.nxt()
                    for kc in range(8):
                        MM(bk.ap[0:m, :], win.ap[:, kc, c0:c0 + m], nT.ap[:, kc, :], kc == 0, kc == 7,
                           [win.r, nT.r], [bk.r])
                    if nm == "gq":
                        ACT(gqT.ap[:, j, :], bk.ap, AF.Copy, [bk.r], [gqT.r])
                    elif nm == "gk":
                        ACT(gkT.ap[:, j, :], bk.ap, AF.Copy, [bk.r], [gkT.r])
                    elif nm == "gl":
                        CPY("dve", glx.ap[0:16, :], bk.ap[0:16, :], [bk.r], [glx.r])
                    elif nm == "cq":
                        TS("dve", cqT.ap[:, j, :], bk.ap, cp[:, C_QN + j:C_QN + j + 1], None, ALU.mult, None,
                           [bk.r, r_cp], [cqT.r])
                    else:
                        TS("dve", ckvT.ap, bk.ap, cp[:, C_KVN:C_KVN + 1], None, ALU.mult, None,
                           [bk.r, r_cp], [ckvT.r])
        def prep_b(s, g):
            RINGS["TPB"] = Ring(banks[0:2])
            RINGS["RB"] = Ring(banks[2:8])
            Sbf_cur = seqst[s]["Sbf"]
            qTg = qTg_r.nxt()
            mixTM = mixTM_r.nxt()
            gctx[(s, g)] = (qTg, mixTM)
            if True:
                for t in range(4):
                    i = 4 * g + t
                    c0 = t * 128
                    c1 = c0 + 128

                    def tmproj(n0, n):
                        bk = RB.nxt()
                        for kc in range(8):
                            MM(bk.ap[:, 0:n], nT.ap[:, kc, c0:c1], win.ap[:, kc, n0:n0 + n], kc == 0, kc == 7,
                               [win.r, nT.r], [bk.r])
                        return bk

                    b_gk = tmproj(256, 256)
                    gktm = gktm_r.nxt()
                    ACT(gktm.ap, b_gk.ap[:, 0:256], AF.Copy, [b_gk.r], [gktm.r])
                    b_gv = tmproj(512, 512)
                    vtm = vtm_r.nxt()
                    ACT(vtm.ap, b_gv.ap, AF.Copy, [b_gv.r], [vtm.r])
                    b_gr = tmproj(1040, 512)
                    sge = sge_r.nxt()
                    ACT(sge.ap, b_gr.ap, AF.Exp, [b_gr.r], [sge.r], scale=-1.0)
                    TS("dve", sge.ap, sge.ap, 1.0, None, ALU.add, None, [sge.r], [sge.r])
                    RCP(sge.ap, sge.ap, [sge.r], [sge.r])
                    TT("dve", sge.ap, sge.ap, b_gr.ap, ALU.mult, [sge.r, b_gr.r], [sge.r])
                    b_c = tmproj(1552, 416)
                    st = smalloc(4)
                    ACT(sq32.ap[:, 0:256], b_c.ap[:, 0:256], AF.Square, [b_c.r], [sq32.r, st.r], accum_out=st.ap[:, 0:1])
                    ACT(sq32.ap[:, 256:384], b_c.ap[:, 256:384], AF.Square, [b_c.r], [sq32.r, st.r], accum_out=st.ap[:, 1:2])
                    ACT(sq32.ap[:, 384:416], b_c.ap[:, 384:416], AF.Square, [b_c.r], [sq32.r, st.r], accum_out=st.ap[:, 2:3])
                    kr = krt.ap[:, 0, :]
                    CPY("dve", kr, b_c.ap[:, 384:416], [b_c.r], [krt.r])

                    b_la = RB.nxt()
                    MM(b_la.ap[:, 0:256], glx.ap[0:32, c0:c1], w2ext, True, True, [glx.r, r_cp], [b_la.r])
                    spb = sp_r.nxt()
                    ACT(spb.ap, b_la.ap[:, 0:256], AF.Exp, [b_la.r], [spb.r], scale=-1.0)
                    ACT(spb.ap, spb.ap, AF.Ln, [spb.r], [spb.r], bias=1.0)
                    b_c2 = RB.nxt()
                    for hp in range(2):
                        MM(b_c2.ap[:, hp * 128:(hp + 1) * 128], spb.ap[:, hp * 128:(hp + 1) * 128], triu, True, True,
                           [spb.r, r_cp], [b_c2.r])
                    MM(b_c2.ap[:, 256:512], lstrict, spb.ap, True, True, [spb.r, r_cp], [b_c2.r])
                    c2v = b_c2.ap[:, 0:256].rearrange("p (a b) -> p a b", a=2)
                    ACT(eb_b.ap, c2v, AF.Exp, [b_c2.r], [eb_b.r], scale=-1.0 / 16)
                    ACT(einv_b.ap, c2v, AF.Exp, [b_c2.r], [einv_b.r], scale=1.0 / 16)
                    ACT(erb_b.ap, b_c2.ap[:, 256:512], AF.Exp, [b_c2.r], [erb_b.r], scale=-1.0 / 16)
                    qeT = qeT_r.nxt()
                    kdT = kdT_r.nxt()
                    kdec = kdec_r.nxt()
                    STT("dve", qeT.ap, gqT.ap[:, :, c0:c1], 0.125, eb_b.ap, ALU.mult, ALU.mult,
                        [gqT.r, eb_b.r], [qeT.r])
                    for r in range(2):
                        sl = slice(r * 64, (r + 1) * 64)
                        TT("dve", kdT.ap[sl, :, r, :], gkT.ap[sl, :, c0:c1], einv_b.ap[sl, :, :], ALU.mult,
                           [gkT.r, einv_b.r], [kdT.r])
                    TT("dve", kdec.ap, gktm.ap, erb_b.ap, ALU.mult, [gktm.r, erb_b.r], [kdec.r])
                    b_at = RB.nxt()
                    atv = b_at.ap.rearrange("p (a b) -> p a b", a=4)
                    for h in range(4):
                        hp, r = h // 2, h % 2
                        MM(atv[:, h, :], kdT.ap[:, hp, r, :], qeT.ap[:, hp, :],
                           True, True, [kdT.r, qeT.r], [b_at.r])
                    ATm = ATm_r.nxt()
                    TT("dve", ATm.ap, atv, triu[:, None, :].to_broadcast([128, 4, 128]), ALU.mult,
                       [b_at.r, r_cp], [ATm.r])
                    b_o = RB.nxt()
                    ov = b_o.ap.rearrange("p (a b) -> p a b", a=4)
                    sbf = Sbf_cur[0]
                    for h in range(4):
                        hp, r = h // 2, h % 2
                        MM(ov[:, h, :], ATm.ap[:, h, :], vtm.ap[:, h * 128:(h + 1) * 128], True, False,
                           [ATm.r, vtm.r], [b_o.r])
                        MM(ov[:, h, :], qeT.ap[:, hp, :], sbf.ap[:, hp, r, :],
                           False, True, [qeT.r, sbf.r], [b_o.r])
                    b_w = RB.nxt()
                    wv = b_w.ap.rearrange("p (a b) -> p a b", a=4)
                    for h in range(4):
                        hp = h // 2
                        MM(wv[:, h, :], kdec.ap[:, hp * 128:(hp + 1) * 128], vtm.ap[:, h * 128:(h + 1) * 128],
                           True, True, [kdec.r, vtm.r], [b_w.r])
                    for h in range(4):
                        hp, r = h // 2, h % 2
                        sl = slice(r * 64, (r + 1) * 64)
                        STT("dve", Sst.ap[sl, hp, :], Sst.ap[sl, hp, :], eb_b.ap[sl, hp, 127:128], wv[sl, h, :],
                            ALU.mult, ALU.add, [Sst.r, eb_b.r, b_w.r], [Sst.r])
                    sbn = Sbf_r.nxt()
                    for r in range(2):
                        sl = slice(r * 64, (r + 1) * 64)
                        CPY("pool", sbn.ap[sl, :, r, :], Sst.ap[sl, :, :], [Sst.r], [sbn.r])
                    Sbf_cur[0] = sbn
                    ssg = smalloc(4)
                    ACT(sq32.ap[:, 0:512], b_o.ap, AF.Square, [b_o.r], [sq32.r])
                    RED(ssg.ap, sq32.ap[:, 0:512].rearrange("p (a b) -> p a b", a=4), [sq32.r], [ssg.r])
                    rstd_inplace(ssg.ap, ssg.r, 1.0 / 128)
                    TT("dve", og32.ap, ov, ssg.ap[:, :, None].to_broadcast([128, 4, 128]), ALU.mult,
                       [b_o.r, ssg.r], [og32.r])
                    TT("pool", og32.ap, og32.ap, gon_b[:, None, :].to_broadcast([128, 4, 128]), ALU.mult,
                       [og32.r, r_cp], [og32.r])
                    TT("pool", mixTM.ap[:, t, 0:512], og32.ap.rearrange("p a b -> p (a b)"), sge.ap, ALU.mult,
                       [og32.r, sge.r], [mixTM.r])

                    TT("dve", st.ap[:, 0:2], st.ap[:, 0:2], cp[:, C_INVN:C_INVN + 2], ALU.mult, [st.r, r_cp], [st.r])
                    ACT(st.ap[:, 0:2], st.ap[:, 0:2], AF.Ln, [st.r, r_eps], [st.r], bias=epsb[:, 0:1])
                    ACT(st.ap[:, 0:2], st.ap[:, 0:2], AF.Exp, [st.r], [st.r], scale=-0.5)
                    rsq = st.ap[:, 0:1]
                    rskv = st.ap[:, 1:2]
                    b_q0 = RB.nxt()
                    b_q1 = RB.nxt()
                    for j in range(2):
                        MM(b_q0.ap[:, 0:480], cqT.ap[:, j, c0:c1], wuq.ap[:, j, 0:480], j == 0, j == 1,
                           [cqT.r, wuq.r], [b_q0.r])
                    for j in range(2):
                        MM(b_q1.ap[:, 0:288], cqT.ap[:, j, c0:c1], wuq.ap[:, j, 480:768], j == 0, j == 1,
                           [cqT.r, wuq.r], [b_q1.r])
                    q0v = b_q0.ap[:, 0:480].rearrange("p (a b) -> p a b", a=5)
                    q1v = b_q1.ap[:, 0:288].rearrange("p (a b) -> p a b", a=3)
                    ACT(sq32.ap[:, 0:480], b_q0.ap[:, 0:480], AF.Square, [b_q0.r], [sq32.r])
                    ACT(sq32.ap[:, 480:768], b_q1.ap[:, 0:288], AF.Square, [b_q1.r], [sq32.r])
                    sq8 = smalloc(8)
                    RED(sq8.ap, sq32.ap.rearrange("p (a b) -> p a b", a=8), [sq32.r], [sq8.r])
                    c1s = smalloc(1)
                    TS("dve", c1s.ap, rsq, rsq, 1.0 / 96, ALU.mult, ALU.mult, [st.r], [c1s.r])
                    ACT(sq8.ap, sq8.ap, AF.Ln, [sq8.r, c1s.r, r_eps], [sq8.r], scale=c1s.ap, bias=epsb[:, 0:1])
                    ACT(sq8.ap, sq8.ap, AF.Exp, [sq8.r], [sq8.r], scale=-0.5)
                    TS("dve", sq8.ap, sq8.ap, rsq, float(96 ** -0.5), ALU.mult, ALU.mult, [sq8.r, st.r], [sq8.r])
                    TT("dve", qtmp.ap[:, 0:5, :], q0v, gq_b[:, None, :].to_broadcast([128, 5, 96]), ALU.mult,
                       [b_q0.r, r_cp], [qtmp.r])
                    TT("dve", qtmp.ap[:, 5:8, :], q1v, gq_b[:, None, :].to_broadcast([128, 3, 96]), ALU.mult,
                       [b_q1.r, r_cp], [qtmp.r])
                    TT("pool", qtmp.ap, qtmp.ap, sq8.ap[:, :, None].to_broadcast([128, 8, 96]), ALU.mult,
                       [qtmp.r, sq8.r], [qtmp.r])
                    CPY("pool", qn_b.ap[:, :, 0:64], qtmp.ap[:, :, 0:64], [qtmp.r], [qn_b.r])
                    cb = cosT.ap[:, i, :][:, None, :].to_broadcast([128, 8, 16])
                    sb_ = sinT.ap[:, i, :][:, None, :].to_broadcast([128, 8, 16])
                    x1 = qtmp.ap[:, :, 64:80]
                    x2 = qtmp.ap[:, :, 80:96]
                    r0, r1, r2, r3 = (rtmp.ap[:, k, :, :] for k in range(4))
                    TT("pool", r0, x1, cb, ALU.mult, [qtmp.r, cosT.r], [rtmp.r])
                    TT("pool", r1, x2, sb_, ALU.mult, [qtmp.r, sinT.r], [rtmp.r])
                    TT("pool", r2, x2, cb, ALU.mult, [qtmp.r, cosT.r], [rtmp.r])
                    TT("pool", r3, x1, sb_, ALU.mult, [qtmp.r, sinT.r], [rtmp.r])
                    TT("pool", qn_b.ap[:, :, 64:80], r0, r1, ALU.subtract, [rtmp.r], [qn_b.r])
                    TT("pool", qn_b.ap[:, :, 80:96], r2, r3, ALU.add, [rtmp.r], [qn_b.r])
                    b_k0 = RB.nxt()
                    b_k1 = RB.nxt()
                    MM(b_k0.ap, ckvT.ap[:, c0:c1], wukv.ap[:, 0:512], True, True, [ckvT.r, wukv.r], [b_k0.r])
                    MM(b_k1.ap, ckvT.ap[:, c0:c1], wukv.ap[:, 512:1024], True, True, [ckvT.r, wukv.r], [b_k1.r])
                    k0v = b_k0.ap.rearrange("p (a b) -> p a b", a=4)
                    k1v = b_k1.ap.rearrange("p (a b) -> p a b", a=4)
                    sqk = sq32.ap[:, 0:512].rearrange("p (a b) -> p a b", a=8)
                    ACT(sqk[:, 0:4, :], k0v[:, :, 0:64], AF.Square, [b_k0.r], [sq32.r])
                    ACT(sqk[:, 4:8, :], k1v[:, :, 0:64], AF.Square, [b_k1.r], [sq32.r])
                    sk8 = smalloc(8)
                    RED(sk8.ap, sqk, [sq32.r], [sk8.r])
                    c2s = smalloc(1)
                    TS("dve", c2s.ap, rskv, rskv, None, ALU.mult, None, [st.r], [c2s.r])
                    TS("dve", sk8.ap, sk8.ap, c2s.ap, st.ap[:, 2:3], ALU.mult, ALU.add, [sk8.r, c2s.r, st.r], [sk8.r])
                    ACT(sk8.ap, sk8.ap, AF.Ln, [sk8.r, r_eps], [sk8.r], scale=1.0 / 96, bias=epsb[:, 0:1])
                    ACT(sk8.ap, sk8.ap, AF.Exp, [sk8.r], [sk8.r], scale=-0.5)
                    fkn = smalloc(8)
                    TS("dve", fkn.ap, sk8.ap, rskv, None, ALU.mult, None, [sk8.r, st.r], [fkn.r])
                    TT("dve", ktmp.ap[:, 0:4, :], k0v[:, :, 0:64], gk_b[:, None, 0:64].to_broadcast([128, 4, 64]),
                       ALU.mult, [b_k0.r, r_cp], [ktmp.r])
                    TT("dve", ktmp.ap[:, 4:8, :], k1v[:, :, 0:64], gk_b[:, None, 0:64].to_broadcast([128, 4, 64]),
                       ALU.mult, [b_k1.r, r_cp], [ktmp.r])
                    TT("pool", kn_b.ap[:, :, 0:64], ktmp.ap, fkn.ap[:, :, None].to_broadcast([128, 8, 64]), ALU.mult,
                       [ktmp.r, fkn.r], [kn_b.r])
                    krg = krt.ap[:, 1, :]
                    TT("pool", krg, kr, gk_b[:, 64:96], ALU.mult, [krt.r, r_cp], [krt.r])
                    c16 = cosT.ap[:, i, :]
                    s16 = sinT.ap[:, i, :]
                    t0, t1, t2, t3 = (krt.ap[:, 2 + k // 2, (k % 2) * 16:(k % 2) * 16 + 16] for k in range(4))
                    krr = krt.ap[:, 4, :]
                    TT("pool", t0, krg[:, 0:16], c16, ALU.mult, [krt.r, cosT.r], [krt.r])
                    TT("pool", t1, krg[:, 16:32], s16, ALU.mult, [krt.r, sinT.r], [krt.r])
                    TT("pool", t2, krg[:, 16:32], c16, ALU.mult, [krt.r, cosT.r], [krt.r])
                    TT("pool", t3, krg[:, 0:16], s16, ALU.mult, [krt.r, sinT.r], [krt.r])
                    TT("pool", krr[:, 0:16], t0, t1, ALU.subtract, [krt.r], [krt.r])
                    TT("pool", krr[:, 16:32], t2, t3, ALU.add, [krt.r], [krt.r])
                    TT("pool", kn_b.ap[:, :, 64:96], krr[:, None, :].to_broadcast([128, 8, 32]),
                       sk8.ap[:, :, None].to_broadcast([128, 8, 32]), ALU.mult, [krt.r, sk8.r], [kn_b.r])
                    ACT(Vc.ap[:, i, 0:4, 0:64], k0v[:, :, 64:128], AF.Copy, [b_k0.r, st.r], [Vc_res[i]], scale=rskv)
                    ACT(Vc.ap[:, i, 4:8, 0:64], k1v[:, :, 64:128], AF.Copy, [b_k1.r, st.r], [Vc_res[i]], scale=rskv)
                    tq = TPB.nxt()
                    tqv = bf8(tq)
                    for h in range(8):
                        TR(tqv[0:96, h, :], qn_b.ap[:, h, :], [qn_b.r], [tq.r])
                    CPY("dve", qTg.ap[0:96, :, c0:c1], tqv[0:96, :, :], [tq.r], [qTg.r])
                    tk = TPB.nxt()
                    tkv = bf8(tk)
                    for h in range(8):
                        TR(tkv[0:96, h, :], kn_b.ap[:, h, :], [kn_b.r], [tk.r])
                    ACT(kTc.ap[0:96, :, i * 128:(i + 1) * 128], tkv[0:96, :, :], AF.Copy, [tk.r], [kT_res[i]])

        def attention(s, g, merged=False):
            if merged:
                RINGS["SB"] = Ring(banks[3:6])
                RINGS["ACB"] = Ring(banks[6:8])
            else:
                RINGS["SB"] = Ring(banks[0:4])
                RINGS["ACB"] = Ring(banks[4:6])
            qTg, mixTM = gctx[(s, g)]
            if True:
                nk = 4 * g + 4
                items = [(h, kt) for h in range(8) for kt in range(nk)]
                accs = {}

                def qk(h, kt):
                    lo = max(0, kt - 4 * g)
                    bs = SB.nxt()
                    MM(bs.ap[:, lo * 128:512], kTc.ap[0:96, h, kt * 128:(kt + 1) * 128],
                       qTg.ap[0:96, h, lo * 128:512], True, True, [kT_res[kt], qTg.r], [bs.r])
                    pbf = pb_r.nxt()
                    ACT(pbf.ap[:, lo * 128:512], bs.ap[:, lo * 128:512], AF.Exp, [bs.r], [pbf.r])
                    if kt >= 4 * g:
                        MS(MSENG, pbf.ap[64:128, lo * 128:lo * 128 + 64], 0.0, [pbf.r])
                    return pbf

                def pv(h, kt, pbf):
                    lo = max(0, kt - 4 * g)
                    if kt == 0:
                        accs[h] = ACB.nxt()
                    acc = accs[h]
                    accv = acc.ap[:, 0:260].rearrange("p (a b) -> p a b", a=4)
                    for qt in range(lo, 4):
                        MM(accv[:, qt, :], pbf.ap[:, qt * 128:(qt + 1) * 128], Vc.ap[:, kt, h, :],
                           kt == 0 and qt == 0, kt == 4 * g + qt, [pbf.r, Vc_res[kt]], [acc.r], skip=True)
                    if kt == nk - 1:
                        rec = smalloc(4)
                        RCP(rec.ap, accv[:, :, 64], [acc.r], [rec.r])
                        TT("dve", mixTM.ap[:, :, 512 + h * 64:512 + (h + 1) * 64], accv[:, :, 0:64],
                           rec.ap[:, :, None].to_broadcast([128, 4, 64]), ALU.mult, [acc.r, rec.r], [mixTM.r])

                LA = 2
                pq = [qk(*items[m]) for m in range(min(LA, len(items)))]
                for n in range(len(items)):
                    if n + LA < len(items):
                        pq.append(qk(*items[n + LA]))
                    pv(items[n][0], items[n][1], pq.pop(0))
                for t in range(4):
                    i = 4 * g + t
                    DMA("sp", mixd[s, i * 128:(i + 1) * 128, :], mixTM.ap[:, t, :], [mixTM.r], [], ch_mixst)

        order = [(sq_, g_) for sq_ in range(2) for g_ in range(4)]

        seq_setup(0)
        prep_a(0, 0, False)
        prep_b(0, 0)
        for k in range(len(order)):
            nxt_ = order[k + 1] if k + 1 < len(order) else None
            if MERGE and nxt_ is not None:
                if nxt_[1] == 0:
                    seq_setup(nxt_[0])
                a_ops = S.capture(lambda: attention(order[k][0], order[k][1], True))
                p_ops = S.capture(lambda: prep_a(nxt_[0], nxt_[1], True))
                S.cur.extend(S.merge(a_ops, p_ops))
                prep_b(*nxt_)
            else:
                attention(order[k][0], order[k][1], False)
                if nxt_ is not None:
                    if nxt_[1] == 0:
                        seq_setup(nxt_[0])
                    prep_a(nxt_[0], nxt_[1], False)
                    prep_b(*nxt_)

        S.barrier()

        RINGS["TPB"] = Ring(banks[0:2])
        RINGS["RB"] = Ring(banks[2:8])
        B = Arena()
        wout = B.take([8, 1024], BF16, "wout")
        wpp = B.take([2, 1024], BF16, "wpp")
        wup_r = B.ring(2, [8, 512], BF16, "wup")
        wdn_r = B.ring(2, [4, 1024], BF16, "wdn")
        mT = B.take([8, SEQ], BF16, "mT")
        hid_r = B.ring(2, [4, 512], BF16, "hid")
        xnb_r = B.ring(2, [1024], BF16, "xnb")
        mixl_r = B.ring(3, [1024], BF16, "mixl")
        mixT_r = B.ring(2, [8, 128], BF16, "mixT")
        hT_r = B.ring(3, [8, 128], BF16, "hT")
        pbf_r = B.ring(3, [256], BF16, "pbf")
        pT_r = B.ring(3, [2, 128], BF16, "pT")
        sqjb = B.take([1024], BF16, "sqjb")
        hres = B.take([NT, 1024], F32, "h")
        rl_r = B.ring(2, [512], F32, "rl")
        eg_r = B.ring(2, [512], F32, "eg")
        eg_r = Ring(eg_r.b + rl_r.b)
        print("phase B arena use (fp32 cols):", B.off, "of", ARN)
        h_r = [Res(f"h{i}") for i in range(NT)]

        w_up_v = w_up.rearrange("(kc p) f -> p kc f", p=128)
        w_dn_v = w_dn.rearrange("(fc p) n -> p fc n", p=128)
        DMA("pool", wpp.ap, w_pp.rearrange("(j p) n -> p j n", p=128), [], [wpp.r])

        def norm_stats(i):
            ss = smalloc(1)
            ACT(sqjb.ap, hres.ap[:, i, :], AF.Square, [h_r[i]], [sqjb.r, ss.r], accum_out=ss.ap)
            rstd_inplace(ss.ap, ss.r, 1.0 / DM)
            return ss

        def norm_apply(i, ss, gain, dstT, dst_r, dst_cols):
            xn = xnb_r.nxt()
            TS("dve", xn.ap, hres.ap[:, i, :], ss.ap, None, ALU.mult, None, [h_r[i], ss.r], [xn.r])
            tp = TPB.nxt()
            tpv = bf8(tp)
            for kc in range(8):
                TR(tpv[:, kc, :], xn.ap[:, kc * 128:(kc + 1) * 128], [xn.r], [tp.r])
            TT("dve", dstT[:, :, dst_cols], tpv, gain[:, :, None].to_broadcast([128, 8, 128]), ALU.mult,
               [tp.r, r_cp], [dst_r])

        def load_chunk(e8):
            wu = wup_r.nxt()
            wd = wdn_r.nxt()
            DMA("pool", wu.ap, w_up_v[:, :, e8 * 512:(e8 + 1) * 512], [], [wu.r], ch_wup)
            DMA("pool", wd.ap, w_dn_v[:, 4 * e8:4 * e8 + 4, :], [], [wd.r], ch_wdn)
            return (wu, wd)

        nxt_chunk = [None]

        def phaseB_seq(s):
            nxt_chunk[0] = load_chunk(0)
            for kc in range(8):
                DMA("pool", wout.ap[:, kc, :], w_out.rearrange("(kc p) n -> p kc n", p=128)[:, kc, :], [], [wout.r], ch_wout)
            b0 = {}

            b0l = {}

            def b0_0(i):
                DMA("sp", hres.ap[:, i, :], x[s, i * 128:(i + 1) * 128, :], [], [h_r[i]], ch_h)
                ml = mixl_r.nxt()
                DMA("sp", ml.ap, mixd[s, i * 128:(i + 1) * 128, :], [], [ml.r], ch_mixl)
                b0l[i] = ml

            def b0_1(i):
                ml = b0l[i]
                tp = TPB.nxt()
                tpv = bf8(tp)
                for kc in range(8):
                    TR(tpv[:, kc, :], ml.ap[:, kc * 128:(kc + 1) * 128], [ml.r], [tp.r])
                mxT = mixT_r.nxt()
                ACT(mxT.ap, tpv, AF.Copy, [tp.r], [mxT.r])
                b0[i] = mxT

            def b0_2(i):
                mxT = b0[i]
                for half in range(2):
                    bk = RB.nxt()
                    for kc in range(8):
                        MM(bk.ap, mxT.ap[:, kc, :], wout.ap[:, kc, half * 512:(half + 1) * 512], kc == 0, kc == 7,
                           [mxT.r, wout.r], [bk.r])
                    hs = hres.ap[:, i, half * 512:(half + 1) * 512]
                    TT("dve", hs, hs, bk.ap, ALU.add, [h_r[i], bk.r], [h_r[i]])

            def b0_3(i):
                b0[i] = norm_stats(i)

            def b0_4(i):
                norm_apply(i, b0[i], g_mn, mT.ap, mT.r, slice(i * 128, (i + 1) * 128))

            for step in range(NT + 5):
                for st_, fn_ in ((0, b0_0), (2, b0_1), (3, b0_2), (4, b0_3), (5, b0_4)):
                    i = step - st_
                    if 0 <= i < NT:
                        fn_(i)
            for e8 in range(8):
                wu, wd = nxt_chunk[0]
                if e8 + 1 < 8:
                    nxt_chunk[0] = load_chunk(e8 + 1)
                if e8 == 0:
                    for kc in range(8):
                        DMA("pool", wout.ap[:, kc, :], w_pg.rearrange("(kc p) n -> p kc n", p=128)[:, kc, :],
                            [], [wout.r], ch_wout)
                for g in range(4):
                    hb = hid_r.nxt()
                    for fc in range(4):
                        bk = RB.nxt()
                        for kc in range(8):
                            MM(bk.ap, wu.ap[:, kc, fc * 128:(fc + 1) * 128], mT.ap[:, kc, g * 512:(g + 1) * 512],
                               kc == 0, kc == 7, [wu.r, mT.r], [bk.r])
                        rl = rl_r.nxt()
                        ACT(rl.ap, bk.ap, AF.Relu, [bk.r], [rl.r])
                        TT("dve", hb.ap[:, fc, :], rl.ap, rl.ap, ALU.mult, [rl.r], [hb.r])
                    for tt in range(4):
                        i = 4 * g + tt
                        for half in range(2):
                            bk = RB.nxt()
                            for fc in range(4):
                                MM(bk.ap, hb.ap[:, fc, tt * 128:(tt + 1) * 128],
                                   wd.ap[:, fc, half * 512:(half + 1) * 512], fc == 0, fc == 3, [hb.r, wd.r], [bk.r])
                            hs = hres.ap[:, i, half * 512:(half + 1) * 512]
                            TT("dve", hs, hs, bk.ap, ALU.add, [h_r[i], bk.r], [h_r[i]])
            pl = {}

            def ple_1(i):
                pb_ = pbf_r.nxt()
                DMA("pool", pb_.ap, p_in[s, i * 128:(i + 1) * 128, :], [], [pb_.r], ch_p)
                pl[i] = [norm_stats(i), pb_]

            def ple_2(i):
                hT = hT_r.nxt()
                pb_ = pl[i][1]
                norm_apply(i, pl[i][0], g_pn, hT.ap, hT.r, slice(0, 128))
                tp = TPB.nxt()
                tpv = bf8(tp)
                for j in range(2):
                    TR(tpv[:, j, :], pb_.ap[:, j * 128:(j + 1) * 128], [pb_.r], [tp.r])
                pT = pT_r.nxt()
                ACT(pT.ap, tpv[:, 0:2, :], AF.Copy, [tp.r], [pT.r])
                pl[i] = [hT, pT]

            def ple_3(i):
                hT, pT = pl[i][0], pl[i][1]
                egs = []
                for half in range(2):
                    hsl = slice(half * 512, (half + 1) * 512)
                    bg = RB.nxt()
                    MM(bg.ap, onesb[0:1, :], bplb[0:1, hsl], True, False, [r_ones, r_bpl], [bg.r])
                    for kc in range(8):
                        MM(bg.ap, hT.ap[:, kc, :], wout.ap[:, kc, hsl], False, kc == 7, [hT.r, wout.r], [bg.r])
                    eg = eg_r.nxt()
                    ACT(eg.ap, bg.ap, AF.Exp, [bg.r], [eg.r], scale=-1.0)
                    ACT(eg.ap, eg.ap, AF.Ln, [eg.r], [eg.r], bias=1.0)
                    ACT(eg.ap, eg.ap, AF.Exp, [eg.r], [eg.r], scale=-1.0)
                    egs.append(eg)
                pl[i] = [hT, pT, egs]

            def ple_4(i):
                hT, pT, egs = pl[i]
                for half in range(2):
                    hsl = slice(half * 512, (half + 1) * 512)
                    eg = egs[half]
                    bp = RB.nxt()
                    for j in range(2):
                        MM(bp.ap, pT.ap[:, j, :], wpp.ap[:, j, hsl], j == 0, j == 1, [pT.r, wpp.r], [bp.r])
                    TT("dve", eg.ap, eg.ap, bp.ap, ALU.mult, [eg.r, bp.r], [eg.r])
                    hs = hres.ap[:, i, hsl]
                    TT("pool", hs, hs, eg.ap, ALU.add, [h_r[i], eg.r], [h_r[i]])
                DMA("sp", y[s, i * 128:(i + 1) * 128, :], hres.ap[:, i, :], [h_r[i]], [], ch_y)

            for step in range(NT + 4):
                for st_, fn_ in ((0, ple_1), (2, ple_2), (3, ple_3), (4, ple_4)):
                    i = step - st_
                    if 0 <= i < NT:
                        fn_(i)

        for s in range(2):
            if STOPK >= 0:
                break
            phaseB_seq(s)

        S.finalize()
        print("ops:", len(S.ops), "signals:", S.counts, "chans:", len(S.chans))
        sems = {}
        for k in ENGS:
            sems[k] = es.enter_context(nc.semaphore(f"s_{k}"))
        for ci, c in enumerate(S.chans):
            sems[c] = es.enter_context(nc.semaphore(f"c{ci}"))
        S.emit(nc, sems)
    return nc


def make_cpack(attn_norm, mlp_norm, ple_norm, mla_q_norm, mla_kv_norm, qk_norm_q, qk_norm_k,
               gla_out_norm, gla_gate_w2, gla_gate_b, b_ple_gate):
    c = np.zeros((128, CP), np.float32)
    c[:, C_AN:C_AN + 8] = attn_norm.reshape(8, 128).T
    c[:, C_MN:C_MN + 8] = mlp_norm.reshape(8, 128).T
    c[:, C_PN:C_PN + 8] = ple_norm.reshape(8, 128).T
    c[:, C_QN:C_QN + 2] = mla_q_norm.reshape(2, 128).T
    c[:, C_KVN] = mla_kv_norm.reshape(128)
    c[:, C_INVN] = 1.0 / 256
    c[:, C_INVN + 1] = 1.0 / 128
    c[:, C_GQ:C_GQ + 96] = qk_norm_q.reshape(1, 96)
    c[:, C_GK:C_GK + 96] = qk_norm_k.reshape(1, 96)
    c[:, C_GON:C_GON + 128] = gla_out_norm.reshape(1, 128)
    invf = (10000.0 ** (-np.arange(0, 32, 2, dtype=np.float32) / np.float32(32))).astype(np.float32)
    c[:, C_INVF:C_INVF + 16] = invf[None, :]
    c[:, C_ID:C_ID + 128] = np.eye(128, dtype=np.float32)
    sidx = np.arange(128)[:, None]
    tidx = np.arange(128)[None, :]
    c[:, C_TRIU:C_TRIU + 128] = (sidx <= tidx).astype(np.float32)
    c[:, C_LST:C_LST + 128] = (sidx > tidx).astype(np.float32)
    c[0:16, C_W2:C_W2 + 256] = gla_gate_w2.reshape(16, 256)
    c[16, C_W2:C_W2 + 256] = gla_gate_b.reshape(256)
    return c


_NC_CACHE = {}


def kernel(x, p, positions, attn_norm, w_in, gla_gate_w2, gla_gate_b, gla_out_norm,
           mla_q_norm, mla_w_uq, mla_kv_norm, mla_w_ukv, qk_norm_q, qk_norm_k,
           w_out, mlp_norm, w_mlp_up, w_mlp_down, ple_norm, w_ple_gate, b_ple_gate,
           w_ple_proj):
    f = lambda a: np.ascontiguousarray(np.asarray(a, dtype=np.float32))
    x = f(x)
    p = f(p)[0]
    positions = np.ascontiguousarray(np.asarray(positions, dtype=np.int32))
    cpack = make_cpack(f(attn_norm), f(mlp_norm), f(ple_norm), f(mla_q_norm), f(mla_kv_norm),
                       f(qk_norm_q), f(qk_norm_k), f(gla_out_norm), f(gla_gate_w2), f(gla_gate_b),
                       f(b_ple_gate))
    shared = {
        "cpack": cpack, "bple": f(b_ple_gate).reshape(1, 1024),
        "w_in": f(w_in)[0], "w_uq": f(mla_w_uq)[0], "w_ukv": f(mla_w_ukv)[0], "w_out": f(w_out)[0],
        "w_up": f(w_mlp_up)[0], "w_dn": f(w_mlp_down)[0], "w_pg": f(w_ple_gate)[0], "w_pp": f(w_ple_proj)[0],
    }
    in_maps = []
    for c in range(NCORES):
        pos2 = positions[2 * c:2 * c + 2]
        posl = np.ascontiguousarray(pos2.reshape(2, NT, 128).transpose(2, 0, 1))
        m = dict(shared)
        m["x"] = np.ascontiguousarray(x[2 * c:2 * c + 2])
        m["p"] = np.ascontiguousarray(p[2 * c:2 * c + 2])
        m["posl"] = posl
        in_maps.append(m)
    if "nc" not in _NC_CACHE:
        _NC_CACHE["nc"] = build_nc()
    nc = _NC_CACHE["nc"]
    res = run_bass_kernel_spmd(nc, in_maps, core_ids=list(range(NCORES)))
    _NC_CACHE["last"] = res
    out = np.concatenate([np.asarray(r["y"]) for r in res.results], axis=0)
    return out.astype(np.float32)
```
